# Optimizing a Trainium2 kernel written in Bass

```python
import math
import jax, jax.numpy as jnp
from jax import lax
import numpy as np

D_MODEL = 2048
BATCH = 8
SEQ = 2048
DEPTH = 1

RNN_WIDTH = D_MODEL
RNN_BLOCKS = 16
RNN_BLOCK = RNN_WIDTH // RNN_BLOCKS
CONV_WIDTH = 4
RG_C = 8.0
HEAD_DIM = 128
ATTN_HEADS = D_MODEL // HEAD_DIM
KV_HEADS = 4
GROUPS = ATTN_HEADS // KV_HEADS
Q_WIDTH = ATTN_HEADS * HEAD_DIM
KV_WIDTH = KV_HEADS * HEAD_DIM
IDX_HEADS = 16
IDX_DIM = 64
IDX_Q_WIDTH = IDX_HEADS * IDX_DIM
TOPK_MAX = 256
Q_BLOCK = 128
NUM_BUCKETS = 32
MAX_DISTANCE = 128
D_FF = 5504
RMS_EPS = 1e-6

IN_SPLITS = (RNN_WIDTH, RNN_WIDTH, Q_WIDTH, KV_WIDTH, KV_WIDTH, IDX_Q_WIDTH, IDX_DIM, IDX_HEADS, D_MODEL, D_MODEL)
N_IN = 2 * RNN_WIDTH + Q_WIDTH + 2 * KV_WIDTH + IDX_Q_WIDTH + IDX_DIM + IDX_HEADS + 2 * D_MODEL

kernel_name = "hybrid_rglru_dsa_macaron"


def rms_norm(x, g):
    xf = x.astype(jnp.float32)
    y = xf * lax.rsqrt(jnp.mean(xf * xf, axis=-1, keepdims=True) + RMS_EPS)
    return (y * g.astype(jnp.float32)).astype(x.dtype)


def swiglu(x, w_gate, w_up, w_down):
    return (jax.nn.silu(x @ w_gate) * (x @ w_up)) @ w_down


def causal_depthwise_conv(x, w, b):
    s = x.shape[1]
    xp = jnp.pad(x, ((0, 0), (CONV_WIDTH - 1, 0), (0, 0)))
    y = b
    for k in range(CONV_WIDTH):
        y = y + xp[:, k:k + s] * w[k]
    return y


def rg_lru(x, w_a, b_a, w_x, b_x, lam):
    bsz, s, w = x.shape
    xb = x.reshape(bsz, s, RNN_BLOCKS, RNN_BLOCK)
    r = jax.nn.sigmoid(jnp.einsum('bsnc,ncd->bsnd', xb, w_a).reshape(bsz, s, w) + b_a).astype(jnp.float32)
    i = jax.nn.sigmoid(jnp.einsum('bsnc,ncd->bsnd', xb, w_x).reshape(bsz, s, w) + b_x).astype(jnp.float32)
    log_a = -RG_C * r * jax.nn.softplus(-lam.astype(jnp.float32))
    a = jnp.exp(log_a)
    mult = jnp.sqrt(-jnp.expm1(2.0 * log_a))
    u = mult * (i * x.astype(jnp.float32))

    def combine(left, right):
        a_l, b_l = left
        a_r, b_r = right
        return a_l * a_r, a_r * b_l + b_r

    _, h = lax.associative_scan(combine, (a, u), axis=1)
    return h.astype(x.dtype)


def t5_causal_bucket(rel):
    n = jnp.maximum(rel, 0)
    max_exact = NUM_BUCKETS // 2
    nf = jnp.maximum(n, 1).astype(jnp.float32)
    large = max_exact + (jnp.log(nf / max_exact) / math.log(MAX_DISTANCE / max_exact)
                         * (NUM_BUCKETS - max_exact)).astype(jnp.int32)
    large = jnp.minimum(large, NUM_BUCKETS - 1)
    return jnp.where(n < max_exact, n, large)


def dsa_attention(q, k, v, iq, ik, iw, rel_bias):
    bsz, s, _ = q.shape
    k_top = min(TOPK_MAX, s // 4)
    nb = s // Q_BLOCK
    k = k.reshape(bsz, s, KV_HEADS, HEAD_DIM)
    v = v.reshape(bsz, s, KV_HEADS, HEAD_DIM)
    q_blk = jnp.moveaxis(q.reshape(bsz, nb, Q_BLOCK, KV_HEADS, GROUPS, HEAD_DIM), 1, 0)
    iq_blk = jnp.moveaxis(iq.reshape(bsz, nb, Q_BLOCK, IDX_HEADS, IDX_DIM), 1, 0)
    iw_blk = jnp.moveaxis((iw * (IDX_HEADS ** -0.5)).reshape(bsz, nb, Q_BLOCK, IDX_HEADS), 1, 0)
    pos_blk = jnp.arange(s, dtype=jnp.int32).reshape(nb, Q_BLOCK)
    key_pos = jnp.arange(s, dtype=jnp.int32)
    scale = HEAD_DIM ** -0.5
    idx_scale = IDX_DIM ** -0.5

    def block_fn(args):
        qb, iqb, iwb, pos = args
        dots = jnp.einsum('bqhd,bsd->bqhs', iqb, ik).astype(jnp.float32) * idx_scale
        scores = jnp.einsum('bqhs,bqh->bqs', jax.nn.relu(dots), iwb.astype(jnp.float32))
        causal = key_pos[None, None, :] <= pos[None, :, None]
        scores = jnp.where(causal, scores, -jnp.inf)
        _, idx = lax.top_k(scores, k_top)
        kg = jax.vmap(lambda kb, ib: kb[ib])(k, idx)
        vg = jax.vmap(lambda vb, ib: vb[ib])(v, idx)
        logits = jnp.einsum('bqhgd,bqshd->bqhgs', qb, kg).astype(jnp.float32) * scale
        rel = pos[None, :, None] - idx
        bias = rel_bias[t5_causal_bucket(rel)].astype(jnp.float32)
        bias = jnp.transpose(bias.reshape(bsz, Q_BLOCK, k_top, KV_HEADS, GROUPS), (0, 1, 3, 4, 2))
        valid = (rel >= 0)[:, :, None, None, :]
        logits = jnp.where(valid, logits + bias, jnp.finfo(jnp.float32).min)
        p = jax.nn.softmax(logits, axis=-1).astype(vg.dtype)
        out = jnp.einsum('bqhgs,bqshd->bqhgd', p, vg)
        return out.reshape(bsz, Q_BLOCK, Q_WIDTH)

    outs = lax.map(block_fn, (q_blk, iq_blk, iw_blk, pos_blk))
    return jnp.moveaxis(outs, 0, 1).reshape(bsz, s, Q_WIDTH)


def hybrid_mixer(h, w_in, conv_w, conv_b, rg_w_a, rg_b_a, rg_w_x, rg_b_x, rg_lambda,
                 rel_bias, w_proj_rnn, w_proj_attn, w_out):
    proj = h @ w_in
    split_pts = [int(p) for p in np.cumsum(IN_SPLITS)[:-1]]
    rx, rgate, q, k, v, iq, ik, iw, g_rnn, g_attn = jnp.split(proj, split_pts, axis=-1)
    xc = causal_depthwise_conv(rx, conv_w, conv_b)
    y_rnn = rg_lru(xc, rg_w_a, rg_b_a, rg_w_x, rg_b_x, rg_lambda) * jax.nn.gelu(rgate)
    y_attn = dsa_attention(q, k, v, iq, ik, iw, rel_bias)
    merged = (jax.nn.sigmoid(g_rnn) * (y_rnn @ w_proj_rnn)
              + jax.nn.sigmoid(g_attn) * (y_attn @ w_proj_attn))
    return merged @ w_out


def setup_inputs(seed: int = 0) -> dict:
    key = jax.random.key(seed)
    ks = jax.random.split(key, 24)
    f32 = jnp.float32

    def nrm(k, shape, fan_in):
        return jax.random.normal(k, shape, f32) * (fan_in ** -0.5)

    def gain(k, shape):
        return 1.0 + 0.02 * jax.random.normal(k, shape, f32)

    u = jax.random.uniform(ks[13], (DEPTH, RNN_WIDTH), f32, 0.9, 0.999)
    a_base = u ** (1.0 / RG_C)
    rg_lambda = jnp.log(a_base) - jnp.log1p(-a_base)
    return {
        "x": jax.random.normal(ks[0], (BATCH, SEQ, D_MODEL), f32),
        "ffn1_norm": gain(ks[1], (DEPTH, D_MODEL)),
        "ffn1_w_gate": nrm(ks[2], (DEPTH, D_MODEL, D_FF), D_MODEL),
        "ffn1_w_up": nrm(ks[3], (DEPTH, D_MODEL, D_FF), D_MODEL),
        "ffn1_w_down": nrm(ks[4], (DEPTH, D_FF, D_MODEL), D_FF),
        "mix_norm": gain(ks[5], (DEPTH, D_MODEL)),
        "w_in": nrm(ks[6], (DEPTH, D_MODEL, N_IN), D_MODEL),
        "conv_w": nrm(ks[7], (DEPTH, CONV_WIDTH, RNN_WIDTH), CONV_WIDTH),
        "conv_b": 0.01 * jax.random.normal(ks[8], (DEPTH, RNN_WIDTH), f32),
        "rg_w_a": nrm(ks[9], (DEPTH, RNN_BLOCKS, RNN_BLOCK, RNN_BLOCK), RNN_BLOCK),
        "rg_b_a": 0.01 * jax.random.normal(ks[10], (DEPTH, RNN_WIDTH), f32),
        "rg_w_x": nrm(ks[11], (DEPTH, RNN_BLOCKS, RNN_BLOCK, RNN_BLOCK), RNN_BLOCK),
        "rg_b_x": 0.01 * jax.random.normal(ks[12], (DEPTH, RNN_WIDTH), f32),
        "rg_lambda": rg_lambda,
        "rel_bias": 0.5 * jax.random.normal(ks[14], (NUM_BUCKETS, ATTN_HEADS), f32),
        "w_proj_rnn": nrm(ks[15], (DEPTH, RNN_WIDTH, D_MODEL), RNN_WIDTH),
        "w_proj_attn": nrm(ks[16], (DEPTH, Q_WIDTH, D_MODEL), Q_WIDTH),
        "w_out": nrm(ks[17], (DEPTH, D_MODEL, D_MODEL), D_MODEL),
        "ffn2_norm": gain(ks[18], (DEPTH, D_MODEL)),
        "ffn2_w_gate": nrm(ks[19], (DEPTH, D_MODEL, D_FF), D_MODEL),
        "ffn2_w_up": nrm(ks[20], (DEPTH, D_MODEL, D_FF), D_MODEL),
        "ffn2_w_down": nrm(ks[21], (DEPTH, D_FF, D_MODEL), D_FF),
        "final_norm": gain(ks[22], (D_MODEL,)),
    }


def reference(x, ffn1_norm, ffn1_w_gate, ffn1_w_up, ffn1_w_down, mix_norm, w_in, conv_w, conv_b,
              rg_w_a, rg_b_a, rg_w_x, rg_b_x, rg_lambda, rel_bias, w_proj_rnn, w_proj_attn, w_out,
              ffn2_norm, ffn2_w_gate, ffn2_w_up, ffn2_w_down, final_norm):
    for l in range(DEPTH):
        x = x + 0.5 * swiglu(rms_norm(x, ffn1_norm[l]), ffn1_w_gate[l], ffn1_w_up[l], ffn1_w_down[l])
        x = x + hybrid_mixer(rms_norm(x, mix_norm[l]), w_in[l], conv_w[l], conv_b[l],
                             rg_w_a[l], rg_b_a[l], rg_w_x[l], rg_b_x[l], rg_lambda[l],
                             rel_bias, w_proj_rnn[l], w_proj_attn[l], w_out[l])
        x = x + 0.5 * swiglu(rms_norm(x, ffn2_norm[l]), ffn2_w_gate[l], ffn2_w_up[l], ffn2_w_down[l])
    return rms_norm(x, final_norm)
```

```python
import os
import numpy as np
import concourse.bass as bass
import concourse.mybir as mybir
from concourse.bass_utils import run_bass_kernel_spmd

F32 = mybir.dt.float32
BF16 = mybir.dt.bfloat16
AF = mybir.ActivationFunctionType
ALU = mybir.AluOpType
AX = mybir.AxisListType
NPASS = 28

D = 2048
SEQ = 2048
T = 512
NT = SEQ // T
DFF = 5504
NFC = DFF // 128
NIN = 12368
C_RX, C_RG, C_Q, C_K, C_V, C_IQ, C_IK, C_IW, C_GR, C_GA = 0, 2048, 4096, 6144, 6656, 7168, 8192, 8256, 8272, 10320
NEG = -1.0e30
ENGS = ("pe", "act", "dve", "pool", "sp")


class Buf:
    __slots__ = ("writer", "readers", "dreaders")

    def __init__(self, pending=None):
        self.writer = None
        self.readers = dict(pending[0]) if pending else {}
        self.dreaders = list(pending[1]) if pending else []


class Inst:
    __slots__ = ("eng", "fn", "deps", "signal", "sigval", "dma_sem", "dma_val", "idx")

    def __init__(self, eng, fn):
        self.eng = eng
        self.fn = fn
        self.deps = []
        self.signal = False
        self.sigval = 0
        self.dma_sem = None
        self.dma_val = 0


class Sched:
    def __init__(self):
        self.q = {e: [] for e in ENGS}
        self.dma_counts = {}
        self.n = 0
        self.pending = ({}, [])

    def fresh(self):
        return Buf(self.pending)

    def retire(self, bufs):
        pr, pd = self.pending
        for b in bufs:
            cands = list(b.readers.values())
            if b.writer is not None:
                cands.append(b.writer)
            cands += b.dreaders
            for c in cands:
                if c.dma_sem is not None:
                    pd.append(c)
                else:
                    o = pr.get(c.eng)
                    if o is None or o.idx < c.idx:
                        pr[c.eng] = c
        if len(pd) > 8:
            del pd[:-8]

    def issue(self, eng, fn, reads=(), writes=(), dma_sem=None):
        inst = Inst(eng, fn)
        inst.idx = self.n
        self.n += 1
        deps = {}
        is_dma = dma_sem is not None

        def same(o):
            return (not is_dma) and o.dma_sem is None and o.eng == eng

        for b in reads:
            w = b.writer
            if w is not None and not (same(w) and eng == "pe"):
                deps[id(w)] = w
        for b in writes:
            w = b.writer
            if w is not None and not same(w):
                deps[id(w)] = w
            for r in b.readers.values():
                if not same(r):
                    deps[id(r)] = r
            for r in b.dreaders:
                deps[id(r)] = r
        inst.deps = list(deps.values())
        for d in inst.deps:
            if d.dma_sem is None:
                d.signal = True
        if is_dma:
            c = self.dma_counts.get(id(dma_sem), 0) + 16
            self.dma_counts[id(dma_sem)] = c
            inst.dma_sem = dma_sem
            inst.dma_val = c
        for b in writes:
            b.writer = inst
            b.readers = {}
            b.dreaders = []
        for b in reads:
            if b.writer is not inst:
                if is_dma:
                    b.dreaders.append(inst)
                else:
                    b.readers[eng] = inst
        self.q[eng].append(inst)
        return inst

    def emit(self, block, esems):
        for e in ENGS:
            c = 0
            for inst in self.q[e]:
                if inst.dma_sem is None and inst.signal:
                    c += 1
                    inst.sigval = c

        def run(ename, engobj):
            seen = {}
            for inst in self.q[ename]:
                for d in inst.deps:
                    if d.dma_sem is not None:
                        sem, val = d.dma_sem, d.dma_val
                    else:
                        sem, val = esems[d.eng], d.sigval
                    k = id(sem)
                    if seen.get(k, 0) >= val:
                        continue
                    seen[k] = val
                    engobj.wait_ge(sem, val)
                bi = inst.fn(engobj)
                if inst.dma_sem is not None:
                    bi.then_inc(inst.dma_sem, 16)
                elif inst.signal:
                    bi.then_inc(esems[ename], 1)

        @block.tensor
        def _(eng):
            run("pe", eng)

        @block.scalar
        def _(eng):
            run("act", eng)

        @block.vector
        def _(eng):
            run("dve", eng)

        @block.gpsimd
        def _(eng):
            run("pool", eng)

        @block.sync
        def _(eng):
            run("sp", eng)


def t5_bucket_table():
    s = np.arange(128, dtype=np.int32)[:, None]
    t = np.arange(128, dtype=np.int32)[None, :]
    out = []
    for delta in (0, 1):
        rel = t - s + 128 * delta
        n = np.maximum(rel, 0)
        nf = np.maximum(n, 1).astype(np.float32)
        large = 16 + (np.log(nf / np.float32(16)) / np.float32(np.log(128 / 16)) * np.float32(16)).astype(np.int32)
        large = np.minimum(large, 31)
        out.append(np.where(n < 16, n, large))
    return np.stack(out, 0)


def build(ntiles=NT, dbg=False):
    nc = bass.Bass("TRN2", target_bir_lowering=False)

    def din(name, shape):
        return nc.dram_tensor(name, list(shape), F32, kind="ExternalInput").ap()

    x_d = din("x", [SEQ, D])
    w1g, w1u, w1d = din("ffn1_w_gate", [D, DFF]), din("ffn1_w_up", [D, DFF]), din("ffn1_w_down", [DFF, D])
    w2g, w2u, w2d = din("ffn2_w_gate", [D, DFF]), din("ffn2_w_up", [D, DFF]), din("ffn2_w_down", [DFF, D])
    w_in = din("w_in", [D, NIN])
    rgwa, rgwx = din("rg_w_a", [16, 128, 128]), din("rg_w_x", [16, 128, 128])
    wpr, wpa, wout = din("w_proj_rnn", [D, D]), din("w_proj_attn", [D, D]), din("w_out", [D, D])
    vecs_d = din("vecs", [128, 12, 16])
    biasT_d = din("biasT", [128, 2, 16, 128])
    cvec_d = din("cvec", [128, 16])
    ident_d = din("ident", [128, 128])
    causal_d = din("causal", [128, 128])
    out_d = nc.dram_tensor("out", [SEQ, D], F32, kind="ExternalOutput").ap()
    if dbg:
        dbg_d = nc.dram_tensor("dbg", [3, 128, 16, T], F32, kind="ExternalOutput").ap()
        dbgb_d = nc.dram_tensor("dbgb", [3, 128, 16, T], BF16, kind="ExternalOutput").ap()

    S = Sched()
    NSLOT = 4
    SLOTW = 256

    from contextlib import ExitStack
    with ExitStack() as es:
        ucnt = [0]

        def uq(name):
            ucnt[0] += 1
            return "%s_u%d" % (name, ucnt[0])

        def sb(name, shape, dt):
            return es.enter_context(nc.sbuf_tensor(uq(name), list(shape), dt))

        def sem(name):
            return es.enter_context(nc.semaphore(name))

        esems = {e: sem("s_" + e) for e in ENGS}
        slot_sems = [sem("d_slot%d" % i) for i in range(NSLOT)]
        d_c = [sem("d_c%d" % i) for i in range(5)]
        d_xin = [sem("d_xin0"), sem("d_xin1")]
        d_out = [sem("d_out0"), sem("d_out1")]
        d_dbg = sem("d_dbg")

        ident_f = sb("ident_f", [128, 128], F32)
        ident_b = sb("ident_b", [128, 128], BF16)
        ones_f = sb("ones_f", [128, 128], F32)
        ones_b = sb("ones_b", [128, 128], BF16)
        causal = sb("causal", [128, 128], F32)
        vecs = sb("vecs", [128, 12, 16], F32)
        nsp8 = sb("nsp8", [128, 16], F32)
        nsp16 = sb("nsp16", [128, 16], F32)
        negc = sb("negc", [128, 16], F32)
        epsT = sb("epsT", [128, 1], F32)
        EB = sb("EB", [128, 2, 16, 128], BF16)
        kT = sb("kT", [128, 4, SEQ], BF16)
        Vtm = sb("Vtm", [128, 16, 512], BF16)
        ikT2 = sb("ikT2", [128, SEQ], BF16)
        hcar = sb("hcar", [128, 16], F32)
        rxhalo = sb("rxhalo", [128, 16, 3], F32)
        xT = sb("xT", [128, 16, T], F32)
        xnT = sb("xnT", [128, 16, T], BF16)
        wslot = [sb("wslot%d" % i, [128, 16 * SLOTW], BF16) for i in range(NSLOT)]
        c30k = sb("c30k", [128, 1], F32)
        sgt = sb("sgt", [128, 2, T], F32)
        ps = es.enter_context(nc.psum_tensor("ps", [128, 7, 512], F32))
        psb = es.enter_context(nc.psum_tensor("psb", [128, 1024], BF16))
        block = es.enter_context(nc.Block())

        B = Buf
        b_const, b_vecs, b_nsp, b_EB, b_causal, b_negc = B(), B(), B(), B(), B(), B()
        b_kT, b_V, b_ik, b_hcar, b_halo = B(), B(), B(), B(), B()
        b_xT = [B() for _ in range(16)]
        b_xn = [B() for _ in range(16)]
        b_slot = [B() for _ in range(NSLOT)]
        b_sgt = [B(), B()]
        b_ps = [B() for _ in range(7)]
        b_psb = B()
        out_bufs = []

        def I(eng, method, reads, writes, *a, **kw):
            return S.issue(eng, lambda e: getattr(e, method)(*a, **kw), reads, writes)

        def DMA(eng, semh, reads, writes, out, in_, **kw):
            return S.issue(eng, lambda e: e.dma_start(out=out, in_=in_, **kw), reads, writes, dma_sem=semh)

        rot = {"bank": 0, "slot": 0}
        slot_gen = [0] * NSLOT

        def bank():
            b = rot["bank"]
            rot["bank"] = (b + 1) % 5
            return b

        class Panel:
            pass

        def load_panel(src3, nk, ncols):
            assert nk * ncols <= 16 * SLOTW
            s = rot["slot"]
            rot["slot"] = (s + 1) % NSLOT
            slot_gen[s] += 1
            p = Panel()
            p.slot, p.gen, p.buf = s, slot_gen[s], b_slot[s]
            p.v = wslot[s][:, 0:nk * ncols].rearrange("p (k n) -> p k n", n=ncols)
            DMA("pool", slot_sems[s], [], [b_slot[s]], p.v, src3)
            return p

        def wpanel(w, k0, nk, c0, ncols):
            return load_panel(w[k0 * 128:(k0 + nk) * 128, c0:c0 + ncols].rearrange("(k p) n -> p k n", p=128), nk, ncols)

        def mm(bk, cols, panel, lhsT, rhs, start, stop, reads):
            if panel is not None:
                assert slot_gen[panel.slot] == panel.gen, "stale weight panel"
                reads = list(reads) + [panel.buf]
            out = ps[:, bk, 0:cols]
            I("pe", "matmul", reads, [b_ps[bk]], out, lhsT=lhsT, rhs=rhs, start=start, stop=stop)

        DMA("sp", d_c[0], [], [b_const], ident_f[:], ident_d)
        DMA("sp", d_c[1], [], [b_causal], causal[:], causal_d)
        DMA("sp", d_c[2], [], [b_vecs], vecs[:], vecs_d)
        DMA("sp", d_c[3], [], [b_negc], negc[:], cvec_d)
        I("dve", "tensor_copy", [b_const], [b_const], out=ident_b[:], in_=ident_f[:])
        I("dve", "memset", [], [b_const], ones_f[:], 1.0)
        I("dve", "memset", [], [b_const], ones_b[:], 1.0)
        I("dve", "memset", [], [b_const], epsT[:], 1.0e-6)
        I("dve", "memset", [], [b_const], c30k[:], -30000.0)
        I("dve", "memset", [], [b_hcar], hcar[:], 0.0)
        I("dve", "memset", [], [b_halo], rxhalo[:], 0.0)
        I("act", "activation", [b_vecs, b_const], [b_nsp], out=nsp8[:], in_=vecs[:, 11, :], func=AF.Exp, scale=-1.0)
        I("act", "activation", [b_nsp, b_const], [b_nsp], out=nsp8[:], in_=nsp8[:], func=AF.Ln, bias=ones_f[:, 0:1])
        I("dve", "tensor_scalar", [b_nsp], [b_nsp], out=nsp16[:], in0=nsp8[:], scalar1=-16.0, scalar2=None, op0=ALU.mult)
        I("dve", "tensor_scalar", [b_nsp], [b_nsp], out=nsp8[:], in0=nsp8[:], scalar1=-8.0, scalar2=None, op0=ALU.mult)
        I("dve", "tensor_scalar", [b_negc], [b_negc], out=negc[:], in0=negc[:], scalar1=-1.0, scalar2=None, op0=ALU.mult)
        with nc.sbuf_tensor(uq("biasT"), [128, 2, 16, 128], F32) as biasT:
            b_bT = S.fresh()
            DMA("sp", d_c[4], [], [b_bT], biasT[:], biasT_d)
            for dl in range(2):
                for h in range(16):
                    I("act", "activation", [b_bT, b_negc], [b_EB], out=EB[:, dl, h, :], in_=biasT[:, dl, h, :], func=AF.Exp,
                      bias=negc[:, h:h + 1])
            S.retire([b_bT])

        def rmsnorm(gidx, out_f32):
            with nc.sbuf_tensor(uq("sq"), [128, 2, T], F32) as sq, nc.sbuf_tensor(uq("rstd"), [128, T], F32) as rstd:
                b_sq = [S.fresh(), S.fresh()]
                b_rstd = S.fresh()
                bk = bank()
                for c in range(16):
                    I("act", "activation", [b_xT[c]], [b_sq[c % 2]], out=sq[:, c % 2, :], in_=xT[:, c, :], func=AF.Square)
                    I("pe", "matmul", [b_sq[c % 2], b_const], [b_ps[bk]], ps[:, bk, :], lhsT=ones_f[:], rhs=sq[:, c % 2, :],
                      start=(c == 0), stop=(c == 15))
                I("act", "activation", [b_ps[bk], b_const], [b_rstd], out=rstd[:], in_=ps[:, bk, :], func=AF.Sqrt, scale=1.0 / D,
                  bias=epsT[:, 0:1])
                I("dve", "reciprocal", [b_rstd], [b_rstd], out=rstd[:], in_=rstd[:])
                for c in range(16):
                    if out_f32:
                        I("dve", "scalar_tensor_tensor", [b_xT[c], b_vecs, b_rstd], [b_xT[c]], out=xT[:, c, :], in0=xT[:, c, :],
                          scalar=vecs[:, gidx, c:c + 1], in1=rstd[:], op0=ALU.mult, op1=ALU.mult)
                    else:
                        I("dve", "scalar_tensor_tensor", [b_xT[c], b_vecs, b_rstd], [b_xn[c]], out=xnT[:, c, :], in0=xT[:, c, :],
                          scalar=vecs[:, gidx, c:c + 1], in1=rstd[:], op0=ALU.mult, op1=ALU.mult)
                S.retire(b_sq + [b_rstd])

        def proj_chunk(panel, j, rhs_tile, rhs_bufs, nk=16, cols=T):
            bk = bank()
            for k in range(nk):
                mm(bk, cols, panel, panel.v[:, k, j * 128:(j + 1) * 128], rhs_tile[:, k, 0:cols], k == 0, k == nk - 1,
                   [rhs_bufs[k]])
            return bk

        def ffn(wg, wu, wd):
            NQ = 11
            with nc.sbuf_tensor(uq("actT"), [128, NQ, T], BF16) as actT:
                b_act = [S.fresh() for _ in range(NQ)]
                for q0 in range(0, NFC, NQ):
                    q1 = min(NFC, q0 + NQ)
                    for p0 in range(q0, q1, 2):
                        n = min(2, q1 - p0)
                        pg = wpanel(wg, 0, 16, p0 * 128, n * 128)
                        pu = wpanel(wu, 0, 16, p0 * 128, n * 128)
                        for j in range(n):
                            fc = p0 + j
                            bg = proj_chunk(pg, j, xnT, b_xn)
                            bu = proj_chunk(pu, j, xnT, b_xn)
                            k2 = fc % 2
                            I("act", "activation", [b_ps[bg]], [b_sgt[k2]], out=sgt[:, k2, :], in_=ps[:, bg, :], func=AF.Silu)
                            I("dve", "tensor_tensor", [b_sgt[k2], b_ps[bu]], [b_act[fc - q0]], out=actT[:, fc - q0, :],
                              in0=sgt[:, k2, :], in1=ps[:, bu, :], op=ALU.mult)
                    nq = q1 - q0
                    for grp in range(8):
                        pd = wpanel(wd, q0, nq, grp * 256, 256)
                        for j in range(2):
                            c = grp * 2 + j
                            bk = bank()
                            for f in range(nq):
                                mm(bk, T, pd, pd.v[:, f, j * 128:(j + 1) * 128], actT[:, f, :], f == 0, f == nq - 1, [b_act[f]])
                            I("dve", "scalar_tensor_tensor", [b_ps[bk], b_xT[c]], [b_xT[c]], out=xT[:, c, :], in0=ps[:, bk, :],
                              scalar=0.5, in1=xT[:, c, :], op0=ALU.mult, op1=ALU.add)
                S.retire(b_act)

        def dump(idx, tile, bufs, bf):
            ob = Buf()
            out_bufs.append(ob)
            DMA("sp", d_dbg, bufs, [ob], (dbgb_d if bf else dbg_d)[idx], tile[:])

        for ti in range(ntiles):
            t0 = ti * T
            with nc.sbuf_tensor(uq("xtm"), [128, 2, D], F32) as xtm:
                b_xtm = [S.fresh(), S.fresh()]
                for tb in range(4):
                    k2 = tb % 2
                    DMA("sp", d_xin[k2], [], [b_xtm[k2]], xtm[:, k2, :], x_d[t0 + tb * 128:t0 + (tb + 1) * 128, :])
                    for cg in range(4):
                        bk = bank()
                        for j in range(4):
                            c = cg * 4 + j
                            I("pe", "transpose", [b_xtm[k2], b_const], [b_ps[bk]], out=ps[:, bk, j * 128:(j + 1) * 128],
                              in_=xtm[:, k2, c * 128:(c + 1) * 128], identity=ident_f[:])
                        I("act", "activation", [b_ps[bk]], b_xT[cg * 4:cg * 4 + 4], out=xT[:, cg * 4:cg * 4 + 4, tb * 128:(tb + 1) * 128],
                          in_=ps[:, bk, :].rearrange("p (c t) -> p c t", c=4), func=AF.Copy)
                S.retire(b_xtm)

            rmsnorm(0, False)
            ffn(w1g, w1u, w1d)
            if dbg and ti == 0:
                dump(0, xT, b_xT, False)

            rmsnorm(1, False)
            es2 = ExitStack()
            yattnT = es2.enter_context(nc.sbuf_tensor(uq("yattnT"), [128, 16, T], BF16))
            b_ya = [S.fresh() for _ in range(16)]

            with ExitStack() as ea:
                def sa(name, shape, dt):
                    return ea.enter_context(nc.sbuf_tensor(uq(name), list(shape), dt))
                qT = sa("qT", [128, 16, T], BF16)
                iqT = sa("iqT", [128, 8, T], BF16)
                iwtm = sa("iwtm", [128, 4, 16], F32)
                acc = sa("acc", [128, SEQ], F32)
                bis = sa("bis", [128, 8], F32)
                mask = sa("mask", [128, SEQ], BF16)
                maskT = sa("maskT", [128, 2, 16, 128], BF16)
                e32 = sa("e32", [128, 512], F32)
                pT = sa("pT", [128, 4, 512], BF16)
                pcnt = [0]
                rs = sa("rs", [128, 512], F32)
                b_q = [S.fresh() for _ in range(16)]
                b_iq = [S.fresh() for _ in range(8)]
                b_iw, b_mask, b_e32, b_rs = (S.fresh() for _ in range(4))
                b_bR, b_bRp, b_blo, b_bnm, b_bcnt, b_bd = (S.fresh() for _ in range(6))
                b_maskT = [S.fresh(), S.fresh()]
                b_acc = [S.fresh() for _ in range(4)]
                b_pT = [S.fresh() for _ in range(4)]
                allb = b_q + b_iq + [b_iw, b_mask, b_e32, b_rs, b_bR, b_bRp, b_blo, b_bnm, b_bcnt, b_bd] + b_maskT + b_acc + b_pT

                for hp in range(2):
                    pk = wpanel(w_in, 0, 16, C_K + hp * 256, 256)
                    for j in range(2):
                        c = hp * 2 + j
                        bk = proj_chunk(pk, j, xnT, b_xn)
                        I("act", "activation", [b_ps[bk]], [b_kT], out=kT[:, c, t0:t0 + T], in_=ps[:, bk, :], func=AF.Copy)
                for hp in range(2):
                    pv = wpanel(w_in, 0, 16, C_V + hp * 256, 256)
                    for tb in range(4):
                        bk = bank()
                        for k in range(16):
                            mm(bk, 256, pv, xnT[:, k, tb * 128:(tb + 1) * 128], pv.v[:, k, :], k == 0, k == 15, [b_xn[k]])
                        I("act", "activation", [b_ps[bk]], [b_V], out=Vtm[:, ti * 4 + tb, hp * 256:(hp + 1) * 256], in_=ps[:, bk, 0:256],
                          func=AF.Copy)
                pik = Panel()
                s_ = rot["slot"]
                rot["slot"] = (s_ + 1) % NSLOT
                slot_gen[s_] += 1
                pik.slot, pik.gen, pik.buf = s_, slot_gen[s_], b_slot[s_]
                pik.v = wslot[s_][:, 0:16 * 144].rearrange("p (k n) -> p k n", n=144)
                ikv = w_in[:, C_IK:C_IK + 64].rearrange("(k p) n -> p k n", p=128)
                iwv = w_in[:, C_IW:C_IW + 16].rearrange("(k p) n -> p k n", p=128)
                DMA("pool", slot_sems[s_], [], [b_slot[s_]], pik.v[:, :, 0:64], ikv)
                DMA("pool", slot_sems[s_], [], [b_slot[s_]], pik.v[:, :, 64:128], ikv)
                DMA("pool", slot_sems[s_], [], [b_slot[s_]], pik.v[:, :, 128:144], iwv)
                bk = proj_chunk(pik, 0, xnT, b_xn)
                I("act", "activation", [b_ps[bk]], [b_ik], out=ikT2[:, t0:t0 + T], in_=ps[:, bk, :], func=AF.Copy)
                for tb in range(4):
                    bk = bank()
                    for k in range(16):
                        mm(bk, 16, pik, xnT[:, k, tb * 128:(tb + 1) * 128], pik.v[:, k, 128:144], k == 0, k == 15, [b_xn[k]])
                    I("act", "activation", [b_ps[bk]], [b_iw], out=iwtm[:, tb, :], in_=ps[:, bk, 0:16], func=AF.Copy, scale=0.03125)
                for hp in range(8):
                    pq = wpanel(w_in, 0, 16, C_Q + hp * 256, 256)
                    for j in range(2):
                        c = hp * 2 + j
                        bk = proj_chunk(pq, j, xnT, b_xn)
                        I("act", "activation", [b_ps[bk]], [b_q[c]], out=qT[:, c, :], in_=ps[:, bk, :], func=AF.Copy,
                          scale=float(128 ** -0.5))
                for hp in range(4):
                    pq = wpanel(w_in, 0, 16, C_IQ + hp * 256, 256)
                    for j in range(2):
                        c = hp * 2 + j
                        bk = proj_chunk(pq, j, xnT, b_xn)
                        I("act", "activation", [b_ps[bk]], [b_iq[c]], out=iqT[:, c, :], in_=ps[:, bk, :], func=AF.Copy)

                def gen_sctk(jj):
                    jb = ti * 4 + jj
                    Sj = (jb + 1) * 128
                    nch = (Sj + 511) // 512
                    mT = maskT[:, jj % 2]
                    bmT = b_maskT[jj % 2]
                    for h in range(16):
                        pb = (h % 2) * 64
                        for cc in range(nch):
                            w = min(512, Sj - cc * 512)
                            bk = bank()
                            I("pe", "matmul", [b_iq[h // 2], b_ik], [b_ps[bk]], ps[:, bk, 0:w],
                              lhsT=iqT[pb:pb + 64, h // 2, jj * 128:(jj + 1) * 128], rhs=ikT2[pb:pb + 64, cc * 512:cc * 512 + w],
                              start=True, stop=True)
                            k2 = (h * nch + cc) % 2
                            I("act", "activation", [b_ps[bk]], [b_sgt[k2]], out=sgt[:, k2, 0:w], in_=ps[:, bk, 0:w], func=AF.Relu)
                            if h == 0:
                                I("dve", "tensor_scalar", [b_sgt[k2], b_iw], [b_acc[cc]], out=acc[:, cc * 512:cc * 512 + w],
                                  in0=sgt[:, k2, 0:w], scalar1=iwtm[:, jj, 0:1], scalar2=None, op0=ALU.mult)
                            else:
                                I("dve", "scalar_tensor_tensor", [b_sgt[k2], b_iw, b_acc[cc]], [b_acc[cc]],
                                  out=acc[:, cc * 512:cc * 512 + w], in0=sgt[:, k2, 0:w], scalar=iwtm[:, jj, h:h + 1],
                                  in1=acc[:, cc * 512:cc * 512 + w], op0=ALU.mult, op1=ALU.add)
                            yield 0.6
                    if jb >= 2:
                        I("dve", "reduce_max", b_acc, [b_bR], out=bis[:, 0:1], in_=acc[:, 0:Sj], axis=AX.X, apply_absolute_value=True)
                        I("dve", "tensor_scalar", [b_bR], [b_bRp], out=bis[:, 1:2], in0=bis[:, 0:1], scalar1=1.0009765625, scalar2=1e-30,
                          op0=ALU.mult, op1=ALU.add)
                        I("dve", "tensor_scalar", [b_bRp], [b_blo], out=bis[:, 2:3], in0=bis[:, 1:2], scalar1=-1.0, scalar2=None,
                          op0=ALU.mult)
                    I("dve", "tensor_tensor", b_acc + [b_causal], b_acc, out=acc[:, jb * 128:(jb + 1) * 128],
                      in0=acc[:, jb * 128:(jb + 1) * 128], in1=causal[:], op=ALU.add)
                    if jb >= 2:
                        for k in range(NPASS):
                            sk = 2.0 ** -k
                            I("dve", "scalar_tensor_tensor", [b_bRp, b_blo], [b_bnm], out=bis[:, 3:4], in0=bis[:, 1:2], scalar=-sk,
                              in1=bis[:, 2:3], op0=ALU.mult, op1=ALU.subtract)
                            I("act", "activation", b_acc + [b_bnm], [b_mask, b_bcnt], out=mask[:, 0:Sj], in_=acc[:, 0:Sj], func=AF.Sign,
                              bias=bis[:, 3:4], scale=1.0, accum_out=bis[:, 4:5])
                            I("dve", "tensor_scalar", [b_bcnt], [b_bd], out=bis[:, 5:6], in0=bis[:, 4:5], scalar1=511.5 - Sj, scalar2=sk,
                              op0=ALU.is_ge, op1=ALU.mult)
                            I("dve", "scalar_tensor_tensor", [b_bd, b_bRp, b_blo], [b_blo], out=bis[:, 2:3], in0=bis[:, 5:6],
                              scalar=bis[:, 1:2], in1=bis[:, 2:3], op0=ALU.mult, op1=ALU.add)
                            yield Sj / 1200.0 + 1.2
                        I("dve", "tensor_scalar", b_acc + [b_blo], [b_mask], out=mask[:, 0:Sj], in0=acc[:, 0:Sj], scalar1=bis[:, 2:3],
                          scalar2=None, op0=ALU.is_ge)
                    else:
                        I("dve", "tensor_scalar", b_acc, [b_mask], out=mask[:, 0:Sj], in0=acc[:, 0:Sj], scalar1=0.5 * NEG,
                          scalar2=None, op0=ALU.is_ge)
                    for i0 in range(0, jb + 1, 8):
                        n = min(8, jb + 1 - i0)
                        for i in range(n):
                            I("pe", "transpose", [b_mask, b_const], [b_psb], out=psb[:, i * 128:(i + 1) * 128],
                              in_=mask[:, (i0 + i) * 128:(i0 + i + 1) * 128], identity=ident_b[:])
                        I("act", "activation", [b_psb, b_const], [bmT], out=mT[:, i0:i0 + n, :],
                          in_=psb[:, 0:n * 128].rearrange("p (i t) -> p i t", i=n), func=AF.Identity, scale=30000.0, bias=c30k[:, 0:1])
                    yield 1.0

                def cost_sctk(jj):
                    jb = ti * 4 + jj
                    Sj = (jb + 1) * 128
                    nch = (Sj + 511) // 512
                    return 16 * nch * 0.6 + (NPASS * (Sj / 1200.0 + 1.2) if jb >= 2 else 0.0) + 1.0

                def gen_att(jj):
                    jb = ti * 4 + jj
                    mT = maskT[:, jj % 2]
                    bmT = b_maskT[jj % 2]
                    for g in range(4):
                        for i in range(jb + 1):
                            dl = jb - i
                            bk = bank()
                            I("pe", "matmul", [b_kT] + b_q[4 * g:4 * g + 4], [b_ps[bk]],
                              ps[:, bk, :].rearrange("p (h t) -> p h t", h=4), lhsT=kT[:, g, i * 128:(i + 1) * 128],
                              rhs=qT[:, 4 * g:4 * g + 4, jj * 128:(jj + 1) * 128], start=True, stop=False)
                            for hh in range(4):
                                I("pe", "matmul", [bmT, b_const], [b_ps[bk]], ps[:, bk, hh * 128:(hh + 1) * 128], lhsT=ident_b[:],
                                  rhs=mT[:, i, :], start=False, stop=(hh == 3))
                            k2 = pcnt[0] % 4
                            pcnt[0] += 1
                            if dl >= 2:
                                I("act", "activation", [b_ps[bk]], [b_pT[k2]], out=pT[:, k2, :], in_=ps[:, bk, :], func=AF.Exp)
                            else:
                                I("act", "activation", [b_ps[bk]], [b_e32], out=e32[:], in_=ps[:, bk, :], func=AF.Exp)
                                I("dve", "tensor_tensor", [b_e32, b_EB], [b_pT[k2]], out=pT[:, k2, :].rearrange("p (h t) -> p h t", h=4),
                                  in0=e32[:].rearrange("p (h t) -> p h t", h=4), in1=EB[:, dl, 4 * g:4 * g + 4, :], op=ALU.mult)
                            I("pe", "matmul", [b_pT[k2], b_V], [b_ps[5]], ps[:, 5, :], lhsT=Vtm[:, i, g * 128:(g + 1) * 128],
                              rhs=pT[:, k2, :], start=(i == 0), stop=(i == jb))
                            I("pe", "matmul", [b_pT[k2], b_const], [b_ps[6]], ps[:, 6, :], lhsT=ones_b[:], rhs=pT[:, k2, :],
                              start=(i == 0), stop=(i == jb))
                            yield 0.9
                        I("dve", "reciprocal", [b_ps[6]], [b_rs], out=rs[:], in_=ps[:, 6, :])
                        I("dve", "tensor_tensor", [b_ps[5], b_rs], b_ya[4 * g:4 * g + 4],
                          out=yattnT[:, 4 * g:4 * g + 4, jj * 128:(jj + 1) * 128],
                          in0=ps[:, 5, :].rearrange("p (h t) -> p h t", h=4), in1=rs[:].rearrange("p (h t) -> p h t", h=4),
                          op=ALU.mult)
                        yield 1.2

                def cost_att(jj):
                    jb = ti * 4 + jj
                    return 4 * ((jb + 1) * 0.9 + 1.2)

                def interleave(ga, ta, gb, tb):
                    da = db = 0.0
                    la, lb = ga is not None, gb is not None
                    while la or lb:
                        if la and (not lb or da / ta <= db / tb):
                            try:
                                da += next(ga)
                            except StopIteration:
                                la = False
                        else:
                            try:
                                db += next(gb)
                            except StopIteration:
                                lb = False

                interleave(gen_sctk(0), cost_sctk(0), None, 1.0)
                for jj in range(1, 4):
                    interleave(gen_sctk(jj), cost_sctk(jj), gen_att(jj - 1), cost_att(jj - 1))
                interleave(None, 1.0, gen_att(3), cost_att(3))
                S.retire(allb)
            if dbg and ti == 0:
                dump(1, yattnT, b_ya, True)

            yrnnT = es2.enter_context(nc.sbuf_tensor(uq("yrnnT"), [128, 16, T], BF16))
            b_yr = [S.fresh() for _ in range(16)]
            with ExitStack() as ea:
                def sa(name, shape, dt):
                    return ea.enter_context(nc.sbuf_tensor(uq(name), list(shape), dt))
                rxp = sa("rxp", [128, 2, T + 3], F32)
                xc = sa("xc", [128, 2, T], F32)
                xcb = sa("xcb", [128, 2, T], BF16)
                rr = sa("rr", [128, 2, T], F32)
                ig = sa("ig", [128, 2, T], F32)
                aa = sa("aa", [128, 2, T], F32)
                a2 = sa("a2", [128, 2, T], F32)
                uu = sa("uu", [128, 2, T], F32)
                h2 = sa("h2", [128, 2, T], F32)
                gl = sa("gl", [128, 2, T], F32)
                b_rxp = [S.fresh(), S.fresh()]
                b_xc = [S.fresh(), S.fresh()]
                b_xcb = [S.fresh(), S.fresh()]
                b_rr, b_ig, b_aa, b_a2, b_uu = ([S.fresh(), S.fresh()] for _ in range(5))
                b_h2 = [S.fresh(), S.fresh()]
                b_gl = [S.fresh(), S.fresh()]
                allb = b_rxp + b_xc + b_xcb + b_rr + b_ig + b_aa + b_a2 + b_uu + b_h2 + b_gl
                for p in range(8):
                    pw = Panel()
                    s_ = rot["slot"]
                    rot["slot"] = (s_ + 1) % NSLOT
                    slot_gen[s_] += 1
                    pw.slot, pw.gen, pw.buf = s_, slot_gen[s_], b_slot[s_]
                    pw.v = wslot[s_][:, 0:512].rearrange("p (k n) -> p k n", n=128)
                    DMA("pool", slot_sems[s_], [], [b_slot[s_]], pw.v[:, 0:2, :], rgwa[2 * p:2 * p + 2].rearrange("n c d -> c n d"))
                    DMA("pool", slot_sems[s_], [], [b_slot[s_]], pw.v[:, 2:4, :], rgwx[2 * p:2 * p + 2].rearrange("n c d -> c n d"))
                    prx = wpanel(w_in, 0, 16, C_RX + p * 256, 256)
                    prg = wpanel(w_in, 0, 16, C_RG + p * 256, 256)
                    for j in range(2):
                        bk = proj_chunk(prx, j, xnT, b_xn)
                        I("act", "activation", [b_ps[bk]], [b_rxp[j]], out=rxp[:, j, 3:T + 3], in_=ps[:, bk, :], func=AF.Copy)
                    for j in range(2):
                        bk = proj_chunk(prg, j, xnT, b_xn)
                        I("act", "activation", [b_ps[bk]], [b_gl[j]], out=gl[:, j, :], in_=ps[:, bk, :], func=AF.Gelu_apprx_tanh)
                    for j in range(2):
                        c = p * 2 + j
                        I("dve", "tensor_copy", [b_halo], [b_rxp[j]], out=rxp[:, j, 0:3], in_=rxhalo[:, c, :])
                        I("dve", "tensor_copy", [b_rxp[j]], [b_halo], out=rxhalo[:, c, :], in_=rxp[:, j, T:T + 3])
                        I("dve", "tensor_scalar", [b_rxp[j], b_vecs], [b_xc[j]], out=xc[:, j, :], in0=rxp[:, j, 0:T],
                          scalar1=vecs[:, 4, c:c + 1], scalar2=vecs[:, 8, c:c + 1], op0=ALU.mult, op1=ALU.add)
                        for k in range(1, 4):
                            I("dve", "scalar_tensor_tensor", [b_rxp[j], b_vecs, b_xc[j]], [b_xc[j]], out=xc[:, j, :],
                              in0=rxp[:, j, k:k + T], scalar=vecs[:, 4 + k, c:c + 1], in1=xc[:, j, :], op0=ALU.mult, op1=ALU.add)
                        I("dve", "tensor_copy", [b_xc[j]], [b_xcb[j]], out=xcb[:, j, :], in_=xc[:, j, :])
                    gbk = []
                    for j in range(2):
                        bkr, bki = bank(), bank()
                        gbk.append((bkr, bki))
                        mm(bkr, T, pw, pw.v[:, j, :], xcb[:, j, :], True, True, [b_xcb[j]])
                        mm(bki, T, pw, pw.v[:, 2 + j, :], xcb[:, j, :], True, True, [b_xcb[j]])
                    for j in range(2):
                        c = p * 2 + j
                        bkr, bki = gbk[j]
                        I("act", "activation", [b_ps[bkr], b_vecs], [b_rr[j]], out=rr[:, j, :], in_=ps[:, bkr, :], func=AF.Sigmoid,
                          bias=vecs[:, 9, c:c + 1])
                        I("act", "activation", [b_ps[bki], b_vecs], [b_ig[j]], out=ig[:, j, :], in_=ps[:, bki, :], func=AF.Sigmoid,
                          bias=vecs[:, 10, c:c + 1])
                    for j in range(2):
                        c = p * 2 + j
                        I("act", "activation", [b_rr[j], b_nsp], [b_aa[j]], out=aa[:, j, :], in_=rr[:, j, :], func=AF.Exp, scale=nsp8[:, c:c + 1])
                        I("act", "activation", [b_rr[j], b_nsp], [b_a2[j]], out=a2[:, j, :], in_=rr[:, j, :], func=AF.Exp, scale=nsp16[:, c:c + 1])
                    for j in range(2):
                        I("act", "activation", [b_a2[j], b_const], [b_a2[j]], out=a2[:, j, :], in_=a2[:, j, :], func=AF.Sqrt, scale=-1.0,
                          bias=ones_f[:, 0:1])
                    for j in range(2):
                        c = p * 2 + j
                        I("dve", "tensor_tensor", [b_ig[j], b_xc[j]], [b_uu[j]], out=uu[:, j, :], in0=ig[:, j, :], in1=xc[:, j, :], op=ALU.mult)
                        I("dve", "tensor_tensor", [b_uu[j], b_a2[j]], [b_uu[j]], out=uu[:, j, :], in0=uu[:, j, :], in1=a2[:, j, :], op=ALU.mult)
                        I("dve", "tensor_tensor_scan", [b_aa[j], b_uu[j], b_hcar], [b_h2[j]], out=h2[:, j, :], data0=aa[:, j, :], data1=uu[:, j, :],
                          initial=hcar[:, c:c + 1], op0=ALU.mult, op1=ALU.add)
                        I("dve", "tensor_copy", [b_h2[j]], [b_hcar], out=hcar[:, c:c + 1], in_=h2[:, j, T - 1:T])
                    for j in range(2):
                        c = p * 2 + j
                        I("dve", "tensor_tensor", [b_gl[j], b_h2[j]], [b_yr[c]], out=yrnnT[:, c, :], in0=gl[:, j, :], in1=h2[:, j, :],
                          op=ALU.mult)
                S.retire(allb)
            if dbg and ti == 0:
                dump(2, yrnnT, b_yr, True)

            with ExitStack() as ea:
                def sa(name, shape, dt):
                    return ea.enter_context(nc.sbuf_tensor(uq(name), list(shape), dt))
                merged = sa("merged", [128, 16, T], BF16)
                sgr = sa("sgr", [128, 2, T], F32)
                sga = sa("sga", [128, 2, T], F32)
                tm1 = sa("tm1", [128, T], F32)
                tm2 = sa("tm2", [128, T], F32)
                b_mg = [S.fresh() for _ in range(16)]
                b_sgr = [S.fresh(), S.fresh()]
                b_sga = [S.fresh(), S.fresh()]
                b_tm1, b_tm2 = S.fresh(), S.fresh()
                allb = b_mg + b_sgr + b_sga + [b_tm1, b_tm2]
                for p in range(8):
                    pgr = wpanel(w_in, 0, 16, C_GR + p * 256, 256)
                    for j in range(2):
                        bk = proj_chunk(pgr, j, xnT, b_xn)
                        I("act", "activation", [b_ps[bk]], [b_sgr[j]], out=sgr[:, j, :], in_=ps[:, bk, :], func=AF.Sigmoid)
                    pga = wpanel(w_in, 0, 16, C_GA + p * 256, 256)
                    for j in range(2):
                        bk = proj_chunk(pga, j, xnT, b_xn)
                        I("act", "activation", [b_ps[bk]], [b_sga[j]], out=sga[:, j, :], in_=ps[:, bk, :], func=AF.Sigmoid)
                    ppr = wpanel(wpr, 0, 16, p * 256, 256)
                    ppa = wpanel(wpa, 0, 16, p * 256, 256)
                    for j in range(2):
                        c = p * 2 + j
                        b1 = proj_chunk(ppr, j, yrnnT, b_yr)
                        b2 = proj_chunk(ppa, j, yattnT, b_ya)
                        I("dve", "tensor_tensor", [b_ps[b1], b_sgr[j]], [b_tm1], out=tm1[:], in0=sgr[:, j, :], in1=ps[:, b1, :], op=ALU.mult)
                        I("dve", "tensor_tensor", [b_ps[b2], b_sga[j]], [b_tm2], out=tm2[:], in0=sga[:, j, :], in1=ps[:, b2, :], op=ALU.mult)
                        I("dve", "tensor_tensor", [b_tm1, b_tm2], [b_mg[c]], out=merged[:, c, :], in0=tm1[:], in1=tm2[:], op=ALU.add)
                for p in range(8):
                    po = wpanel(wout, 0, 16, p * 256, 256)
                    for j in range(2):
                        c = p * 2 + j
                        bk = proj_chunk(po, j, merged, b_mg)
                        I("dve", "tensor_tensor", [b_ps[bk], b_xT[c]], [b_xT[c]], out=xT[:, c, :], in0=xT[:, c, :], in1=ps[:, bk, :],
                          op=ALU.add)
                S.retire(allb)
            S.retire(b_ya + b_yr)
            es2.close()

            rmsnorm(2, False)
            ffn(w2g, w2u, w2d)
            rmsnorm(3, True)

            with nc.sbuf_tensor(uq("otm"), [128, 2, D], F32) as otm:
                b_otm = [S.fresh(), S.fresh()]
                for tb in range(4):
                    k2 = tb % 2
                    for cg in range(4):
                        bk = bank()
                        for j in range(4):
                            c = cg * 4 + j
                            I("pe", "transpose", [b_xT[c], b_const], [b_ps[bk]], out=ps[:, bk, j * 128:(j + 1) * 128],
                              in_=xT[:, c, tb * 128:(tb + 1) * 128], identity=ident_f[:])
                        I("act", "activation", [b_ps[bk]], [b_otm[k2]], out=otm[:, k2, cg * 512:(cg + 1) * 512], in_=ps[:, bk, :],
                          func=AF.Copy)
                    ob = Buf()
                    out_bufs.append(ob)
                    DMA("sp", d_out[k2], [b_otm[k2]], [ob], out_d[t0 + tb * 128:t0 + (tb + 1) * 128, :], otm[:, k2, :])
                S.retire(b_otm)

        I("sp", "nop", out_bufs, [])
        S.emit(block, esems)
    return nc


_BUCKET = t5_bucket_table()


def _prep_inputs(inp):
    f32 = np.float32

    def fm(v):
        return np.ascontiguousarray(np.asarray(v, f32).reshape(16, 128).T)

    vec_list = [inp["ffn1_norm"][0], inp["mix_norm"][0], inp["ffn2_norm"][0], inp["final_norm"],
                inp["conv_w"][0][0], inp["conv_w"][0][1], inp["conv_w"][0][2], inp["conv_w"][0][3],
                inp["conv_b"][0], inp["rg_b_a"][0], inp["rg_b_x"][0], inp["rg_lambda"][0]]
    vecs = np.ascontiguousarray(np.stack([fm(v) for v in vec_list], axis=1))
    rb = np.asarray(inp["rel_bias"], f32)
    biasT = np.ascontiguousarray(np.transpose(rb[_BUCKET], (1, 0, 3, 2)))
    cvec = np.ascontiguousarray(np.broadcast_to(rb[31][None, :], (128, 16)))
    s = np.arange(128)
    causal = np.where(s[None, :] <= s[:, None], 0.0, NEG).astype(f32)
    shared = {
        "ffn1_w_gate": np.asarray(inp["ffn1_w_gate"][0], f32), "ffn1_w_up": np.asarray(inp["ffn1_w_up"][0], f32),
        "ffn1_w_down": np.asarray(inp["ffn1_w_down"][0], f32),
        "ffn2_w_gate": np.asarray(inp["ffn2_w_gate"][0], f32), "ffn2_w_up": np.asarray(inp["ffn2_w_up"][0], f32),
        "ffn2_w_down": np.asarray(inp["ffn2_w_down"][0], f32),
        "w_in": np.asarray(inp["w_in"][0], f32), "rg_w_a": np.asarray(inp["rg_w_a"][0], f32), "rg_w_x": np.asarray(inp["rg_w_x"][0], f32),
        "w_proj_rnn": np.asarray(inp["w_proj_rnn"][0], f32), "w_proj_attn": np.asarray(inp["w_proj_attn"][0], f32),
        "w_out": np.asarray(inp["w_out"][0], f32),
        "vecs": vecs, "biasT": biasT.astype(f32), "cvec": cvec.astype(f32), "ident": np.eye(128, dtype=f32), "causal": causal,
    }
    return shared


def kernel(**inputs):
    ntiles = int(os.environ.get("KNT", NT))
    dbg = bool(int(os.environ.get("KDBG", "0")))
    shared = _prep_inputs(inputs)
    x = np.asarray(inputs["x"], np.float32)
    nc = build(ntiles, dbg)
    in_maps = []
    for b in range(8):
        m = dict(shared)
        m["x"] = np.ascontiguousarray(x[b])
        in_maps.append(m)
    res = run_bass_kernel_spmd(nc, in_maps, core_ids=list(range(8)))
    out = np.stack([np.asarray(r["out"], np.float32) for r in res.results], axis=0)
    if dbg:
        kernel.dbg = res.results[0]
    return out
```

```python
import os
import numpy as np
import concourse.bass as bass
import concourse.mybir as mybir
from concourse.bass_utils import run_bass_kernel_spmd

F32 = mybir.dt.float32
BF16 = mybir.dt.bfloat16
AF = mybir.ActivationFunctionType
ALU = mybir.AluOpType
AX = mybir.AxisListType
NPASS = 28

D = 2048
SEQ = 2048
T = 512
NT = SEQ // T
DFF = 5504
NFC = DFF // 128
NIN = 12368
C_RX, C_RG, C_Q, C_K, C_V, C_IQ, C_IK, C_IW, C_GR, C_GA = 0, 2048, 4096, 6144, 6656, 7168, 8192, 8256, 8272, 10320
NEG = -1.0e30
ENGS = ("pe", "act", "dve", "pool", "sp")


class Buf:
    __slots__ = ("writer", "readers", "dreaders")

    def __init__(self, pending=None):
        self.writer = None
        self.readers = dict(pending[0]) if pending else {}
        self.dreaders = list(pending[1]) if pending else []


class Inst:
    __slots__ = ("eng", "fn", "deps", "signal", "sigval", "dma_sem", "dma_val", "idx")

    def __init__(self, eng, fn):
        self.eng = eng
        self.fn = fn
        self.deps = []
        self.signal = False
        self.sigval = 0
        self.dma_sem = None
        self.dma_val = 0


class Sched:
    def __init__(self):
        self.q = {e: [] for e in ENGS}
        self.dma_counts = {}
        self.n = 0
        self.pending = ({}, [])

    def fresh(self):
        return Buf(self.pending)

    def retire(self, bufs):
        pr, pd = self.pending
        for b in bufs:
            cands = list(b.readers.values())
            if b.writer is not None:
                cands.append(b.writer)
            cands += b.dreaders
            for c in cands:
                if c.dma_sem is not None:
                    pd.append(c)
                else:
                    o = pr.get(c.eng)
                    if o is None or o.idx < c.idx:
                        pr[c.eng] = c
        if len(pd) > 8:
            del pd[:-8]

    def issue(self, eng, fn, reads=(), writes=(), dma_sem=None):
        inst = Inst(eng, fn)
        inst.idx = self.n
        self.n += 1
        deps = {}
        is_dma = dma_sem is not None

        def same(o):
            return (not is_dma) and o.dma_sem is None and o.eng == eng

        def skip(o):
            return same(o) and eng == "pe"

        for b in reads:
            w = b.writer
            if w is not None and not skip(w):
                deps[id(w)] = w
        for b in writes:
            w = b.writer
            if w is not None and not skip(w):
                deps[id(w)] = w
            for r in b.readers.values():
                if not skip(r):
                    deps[id(r)] = r
            for r in b.dreaders:
                deps[id(r)] = r
        inst.deps = list(deps.values())
        for d in inst.deps:
            if d.dma_sem is None:
                d.signal = True
        if is_dma:
            c = self.dma_counts.get(id(dma_sem), 0) + 16
            self.dma_counts[id(dma_sem)] = c
            inst.dma_sem = dma_sem
            inst.dma_val = c
        for b in writes:
            b.writer = inst
            b.readers = {}
            b.dreaders = []
        for b in reads:
            if b.writer is not inst:
                if is_dma:
                    b.dreaders.append(inst)
                else:
                    b.readers[eng] = inst
        self.q[eng].append(inst)
        return inst

    def emit(self, block, esems):
        for e in ENGS:
            c = 0
            for inst in self.q[e]:
                if inst.dma_sem is None and inst.signal:
                    c += 1
                    inst.sigval = c

        def run(ename, engobj):
            seen = {}
            for inst in self.q[ename]:
                for d in inst.deps:
                    if d.dma_sem is not None:
                        sem, val = d.dma_sem, d.dma_val
                    else:
                        sem, val = esems[d.eng], d.sigval
                    k = id(sem)
                    if seen.get(k, 0) >= val:
                        continue
                    seen[k] = val
                    engobj.wait_ge(sem, val)
                bi = inst.fn(engobj)
                if inst.dma_sem is not None:
                    bi.then_inc(inst.dma_sem, 16)
                elif inst.signal:
                    bi.then_inc(esems[ename], 1)

        @block.tensor
        def _(eng):
            run("pe", eng)

        @block.scalar
        def _(eng):
            run("act", eng)

        @block.vector
        def _(eng):
            run("dve", eng)

        @block.gpsimd
        def _(eng):
            run("pool", eng)

        @block.sync
        def _(eng):
            run("sp", eng)


def t5_bucket_table():
    s = np.arange(128, dtype=np.int32)[:, None]
    t = np.arange(128, dtype=np.int32)[None, :]
    out = []
    for delta in (0, 1):
        rel = t - s + 128 * delta
        n = np.maximum(rel, 0)
        nf = np.maximum(n, 1).astype(np.float32)
        large = 16 + (np.log(nf / np.float32(16)) / np.float32(np.log(128 / 16)) * np.float32(16)).astype(np.int32)
        large = np.minimum(large, 31)
        out.append(np.where(n < 16, n, large))
    return np.stack(out, 0)


def build(ntiles=NT, dbg=False):
    nc = bass.Bass("TRN2", target_bir_lowering=False)

    def din(name, shape):
        return nc.dram_tensor(name, list(shape), F32, kind="ExternalInput").ap()

    x_d = din("x", [SEQ, D])
    w1g, w1u, w1d = din("ffn1_w_gate", [D, DFF]), din("ffn1_w_up", [D, DFF]), din("ffn1_w_down", [DFF, D])
    w2g, w2u, w2d = din("ffn2_w_gate", [D, DFF]), din("ffn2_w_up", [D, DFF]), din("ffn2_w_down", [DFF, D])
    w_in = din("w_in", [D, NIN])
    rgwa, rgwx = din("rg_w_a", [16, 128, 128]), din("rg_w_x", [16, 128, 128])
    wpr, wpa, wout = din("w_proj_rnn", [D, D]), din("w_proj_attn", [D, D]), din("w_out", [D, D])
    vecs_d = din("vecs", [128, 12, 16])
    biasT_d = din("biasT", [128, 2, 16, 128])
    cvec_d = din("cvec", [128, 16])
    ident_d = din("ident", [128, 128])
    causal_d = din("causal", [128, 128])
    out_d = nc.dram_tensor("out", [SEQ, D], F32, kind="ExternalOutput").ap()
    if dbg:
        dbg_d = nc.dram_tensor("dbg", [3, 128, 16, T], F32, kind="ExternalOutput").ap()
        dbgb_d = nc.dram_tensor("dbgb", [3, 128, 16, T], BF16, kind="ExternalOutput").ap()

    S = Sched()
    NSLOT = 4
    SLOTW = 256

    from contextlib import ExitStack
    with ExitStack() as es:
        ucnt = [0]

        def uq(name):
            ucnt[0] += 1
            return "%s_u%d" % (name, ucnt[0])

        def sb(name, shape, dt):
            return es.enter_context(nc.sbuf_tensor(uq(name), list(shape), dt))

        def sem(name):
            return es.enter_context(nc.semaphore(name))

        esems = {e: sem("s_" + e) for e in ENGS}
        slot_sems = [sem("d_slot%d" % i) for i in range(NSLOT)]
        d_c = [sem("d_c%d" % i) for i in range(5)]
        d_xin = [sem("d_xin0"), sem("d_xin1")]
        d_out = [sem("d_out0"), sem("d_out1")]
        d_dbg = sem("d_dbg")

        ident_f = sb("ident_f", [128, 128], F32)
        ident_b = sb("ident_b", [128, 128], BF16)
        ones_f = sb("ones_f", [128, 128], F32)
        ones_b = sb("ones_b", [128, 128], BF16)
        causal = sb("causal", [128, 128], F32)
        vecs = sb("vecs", [128, 12, 16], F32)
        nsp8 = sb("nsp8", [128, 16], F32)
        nsp16 = sb("nsp16", [128, 16], F32)
        negc = sb("negc", [128, 16], F32)
        epsT = sb("epsT", [128, 1], F32)
        EB = sb("EB", [128, 2, 16, 128], BF16)
        kT = sb("kT", [128, 4, SEQ], BF16)
        Vtm = sb("Vtm", [128, 16, 512], BF16)
        ikT2 = sb("ikT2", [128, SEQ], BF16)
        hcar = sb("hcar", [128, 16], F32)
        rxhalo = sb("rxhalo", [128, 16, 3], F32)
        xT = sb("xT", [128, 16, T], F32)
        xnT = sb("xnT", [128, 16, T], BF16)
        wslot = [sb("wslot%d" % i, [128, 16 * SLOTW], BF16) for i in range(NSLOT)]
        c30k = sb("c30k", [128, 1], F32)
        sgt = sb("sgt", [128, 2, T], F32)
        ps = es.enter_context(nc.psum_tensor("ps", [128, 7, 512], F32))
        psb = es.enter_context(nc.psum_tensor("psb", [128, 1024], BF16))
        block = es.enter_context(nc.Block())

        B = Buf
        b_const, b_vecs, b_nsp, b_EB, b_causal, b_negc = B(), B(), B(), B(), B(), B()
        b_kT, b_V, b_ik, b_hcar, b_halo = B(), B(), B(), B(), B()
        b_xT = [B() for _ in range(16)]
        b_xn = [B() for _ in range(16)]
        b_slot = [B() for _ in range(NSLOT)]
        b_sgt = [B(), B()]
        b_ps = [B() for _ in range(7)]
        b_psb = B()
        out_bufs = []

        def I(eng, method, reads, writes, *a, **kw):
            return S.issue(eng, lambda e: getattr(e, method)(*a, **kw), reads, writes)

        def DMA(eng, semh, reads, writes, out, in_, **kw):
            return S.issue(eng, lambda e: e.dma_start(out=out, in_=in_, **kw), reads, writes, dma_sem=semh)

        rot = {"bank": 0, "slot": 0}
        slot_gen = [0] * NSLOT

        def bank():
            b = rot["bank"]
            rot["bank"] = (b + 1) % 5
            return b

        class Panel:
            pass

        def load_panel(src3, nk, ncols):
            assert nk * ncols <= 16 * SLOTW
            s = rot["slot"]
            rot["slot"] = (s + 1) % NSLOT
            slot_gen[s] += 1
            p = Panel()
            p.slot, p.gen, p.buf = s, slot_gen[s], b_slot[s]
            p.v = wslot[s][:, 0:nk * ncols].rearrange("p (k n) -> p k n", n=ncols)
            DMA("pool", slot_sems[s], [], [b_slot[s]], p.v, src3)
            return p

        def wpanel(w, k0, nk, c0, ncols):
            return load_panel(w[k0 * 128:(k0 + nk) * 128, c0:c0 + ncols].rearrange("(k p) n -> p k n", p=128), nk, ncols)

        def mm(bk, cols, panel, lhsT, rhs, start, stop, reads):
            if panel is not None:
                assert slot_gen[panel.slot] == panel.gen, "stale weight panel"
                reads = list(reads) + [panel.buf]
            out = ps[:, bk, 0:cols]
            I("pe", "matmul", reads, [b_ps[bk]], out, lhsT=lhsT, rhs=rhs, start=start, stop=stop)

        DMA("sp", d_c[0], [], [b_const], ident_f[:], ident_d)
        DMA("sp", d_c[1], [], [b_causal], causal[:], causal_d)
        DMA("sp", d_c[2], [], [b_vecs], vecs[:], vecs_d)
        DMA("sp", d_c[3], [], [b_negc], negc[:], cvec_d)
        I("dve", "tensor_copy", [b_const], [b_const], out=ident_b[:], in_=ident_f[:])
        I("dve", "memset", [], [b_const], ones_f[:], 1.0)
        I("dve", "memset", [], [b_const], ones_b[:], 1.0)
        I("dve", "memset", [], [b_const], epsT[:], 1.0e-6)
        I("dve", "memset", [], [b_const], c30k[:], -30000.0)
        I("dve", "memset", [], [b_hcar], hcar[:], 0.0)
        I("dve", "memset", [], [b_halo], rxhalo[:], 0.0)
        I("act", "activation", [b_vecs, b_const], [b_nsp], out=nsp8[:], in_=vecs[:, 11, :], func=AF.Exp, scale=-1.0)
        I("act", "activation", [b_nsp, b_const], [b_nsp], out=nsp8[:], in_=nsp8[:], func=AF.Ln, bias=ones_f[:, 0:1])
        I("dve", "tensor_scalar", [b_nsp], [b_nsp], out=nsp16[:], in0=nsp8[:], scalar1=-16.0, scalar2=None, op0=ALU.mult)
        I("dve", "tensor_scalar", [b_nsp], [b_nsp], out=nsp8[:], in0=nsp8[:], scalar1=-8.0, scalar2=None, op0=ALU.mult)
        I("dve", "tensor_scalar", [b_negc], [b_negc], out=negc[:], in0=negc[:], scalar1=-1.0, scalar2=None, op0=ALU.mult)
        with nc.sbuf_tensor(uq("biasT"), [128, 2, 16, 128], F32) as biasT:
            b_bT = S.fresh()
            DMA("sp", d_c[4], [], [b_bT], biasT[:], biasT_d)
            for dl in range(2):
                for h in range(16):
                    I("act", "activation", [b_bT, b_negc], [b_EB], out=EB[:, dl, h, :], in_=biasT[:, dl, h, :], func=AF.Exp,
                      bias=negc[:, h:h + 1])
            S.retire([b_bT])

        def rmsnorm(gidx, out_f32):
            with nc.sbuf_tensor(uq("sq"), [128, 2, T], F32) as sq, nc.sbuf_tensor(uq("rstd"), [128, T], F32) as rstd:
                b_sq = [S.fresh(), S.fresh()]
                b_rstd = S.fresh()
                bk = bank()
                for c in range(16):
                    I("act", "activation", [b_xT[c]], [b_sq[c % 2]], out=sq[:, c % 2, :], in_=xT[:, c, :], func=AF.Square)
                    I("pe", "matmul", [b_sq[c % 2], b_const], [b_ps[bk]], ps[:, bk, :], lhsT=ones_f[:], rhs=sq[:, c % 2, :],
                      start=(c == 0), stop=(c == 15))
                I("act", "activation", [b_ps[bk], b_const], [b_rstd], out=rstd[:], in_=ps[:, bk, :], func=AF.Sqrt, scale=1.0 / D,
                  bias=epsT[:, 0:1])
                I("dve", "reciprocal", [b_rstd], [b_rstd], out=rstd[:], in_=rstd[:])
                for c in range(16):
                    if out_f32:
                        I("dve", "scalar_tensor_tensor", [b_xT[c], b_vecs, b_rstd], [b_xT[c]], out=xT[:, c, :], in0=xT[:, c, :],
                          scalar=vecs[:, gidx, c:c + 1], in1=rstd[:], op0=ALU.mult, op1=ALU.mult)
                    else:
                        I("dve", "scalar_tensor_tensor", [b_xT[c], b_vecs, b_rstd], [b_xn[c]], out=xnT[:, c, :], in0=xT[:, c, :],
                          scalar=vecs[:, gidx, c:c + 1], in1=rstd[:], op0=ALU.mult, op1=ALU.mult)
                S.retire(b_sq + [b_rstd])

        def proj_chunk(panel, j, rhs_tile, rhs_bufs, nk=16, cols=T):
            bk = bank()
            for k in range(nk):
                mm(bk, cols, panel, panel.v[:, k, j * 128:(j + 1) * 128], rhs_tile[:, k, 0:cols], k == 0, k == nk - 1,
                   [rhs_bufs[k]])
            return bk

        def ffn(wg, wu, wd):
            NQ = 11
            with nc.sbuf_tensor(uq("actT"), [128, NQ, T], BF16) as actT:
                b_act = [S.fresh() for _ in range(NQ)]
                for q0 in range(0, NFC, NQ):
                    q1 = min(NFC, q0 + NQ)
                    for p0 in range(q0, q1, 2):
                        n = min(2, q1 - p0)
                        pg = wpanel(wg, 0, 16, p0 * 128, n * 128)
                        pu = wpanel(wu, 0, 16, p0 * 128, n * 128)
                        for j in range(n):
                            fc = p0 + j
                            bg = proj_chunk(pg, j, xnT, b_xn)
                            bu = proj_chunk(pu, j, xnT, b_xn)
                            k2 = fc % 2
                            I("act", "activation", [b_ps[bg]], [b_sgt[k2]], out=sgt[:, k2, :], in_=ps[:, bg, :], func=AF.Silu)
                            I("dve", "tensor_tensor", [b_sgt[k2], b_ps[bu]], [b_act[fc - q0]], out=actT[:, fc - q0, :],
                              in0=sgt[:, k2, :], in1=ps[:, bu, :], op=ALU.mult)
                    nq = q1 - q0
                    for grp in range(8):
                        pd = wpanel(wd, q0, nq, grp * 256, 256)
                        for j in range(2):
                            c = grp * 2 + j
                            bk = bank()
                            for f in range(nq):
                                mm(bk, T, pd, pd.v[:, f, j * 128:(j + 1) * 128], actT[:, f, :], f == 0, f == nq - 1, [b_act[f]])
                            I("dve", "scalar_tensor_tensor", [b_ps[bk], b_xT[c]], [b_xT[c]], out=xT[:, c, :], in0=ps[:, bk, :],
                              scalar=0.5, in1=xT[:, c, :], op0=ALU.mult, op1=ALU.add)
                S.retire(b_act)

        def dump(idx, tile, bufs, bf):
            ob = Buf()
            out_bufs.append(ob)
            DMA("sp", d_dbg, bufs, [ob], (dbgb_d if bf else dbg_d)[idx], tile[:])

        for ti in range(ntiles):
            t0 = ti * T
            with nc.sbuf_tensor(uq("xtm"), [128, 2, D], F32) as xtm:
                b_xtm = [S.fresh(), S.fresh()]
                for tb in range(4):
                    k2 = tb % 2
                    DMA("sp", d_xin[k2], [], [b_xtm[k2]], xtm[:, k2, :], x_d[t0 + tb * 128:t0 + (tb + 1) * 128, :])
                    for cg in range(4):
                        bk = bank()
                        for j in range(4):
                            c = cg * 4 + j
                            I("pe", "transpose", [b_xtm[k2], b_const], [b_ps[bk]], out=ps[:, bk, j * 128:(j + 1) * 128],
                              in_=xtm[:, k2, c * 128:(c + 1) * 128], identity=ident_f[:])
                        I("act", "activation", [b_ps[bk]], b_xT[cg * 4:cg * 4 + 4], out=xT[:, cg * 4:cg * 4 + 4, tb * 128:(tb + 1) * 128],
                          in_=ps[:, bk, :].rearrange("p (c t) -> p c t", c=4), func=AF.Copy)
                S.retire(b_xtm)

            rmsnorm(0, False)
            ffn(w1g, w1u, w1d)
            if dbg and ti == 0:
                dump(0, xT, b_xT, False)

            rmsnorm(1, False)
            es2 = ExitStack()
            yattnT = es2.enter_context(nc.sbuf_tensor(uq("yattnT"), [128, 16, T], BF16))
            b_ya = [S.fresh() for _ in range(16)]

            with ExitStack() as ea:
                def sa(name, shape, dt):
                    return ea.enter_context(nc.sbuf_tensor(uq(name), list(shape), dt))
                qT = sa("qT", [128, 16, T], BF16)
                iqT = sa("iqT", [128, 8, T], BF16)
                iwtm = sa("iwtm", [128, 4, 16], F32)
                acc = sa("acc", [128, SEQ], F32)
                bis = sa("bis", [128, 8], F32)
                mask = sa("mask", [128, SEQ], BF16)
                maskT = sa("maskT", [128, 2, 16, 128], BF16)
                e32 = sa("e32", [128, 512], F32)
                pT = sa("pT", [128, 4, 512], BF16)
                pcnt = [0]
                rs = sa("rs", [128, 512], F32)
                b_q = [S.fresh() for _ in range(16)]
                b_iq = [S.fresh() for _ in range(8)]
                b_iw, b_mask, b_e32, b_rs = (S.fresh() for _ in range(4))
                b_bR, b_bRp, b_blo, b_bnm, b_bcnt, b_bd = (S.fresh() for _ in range(6))
                b_maskT = [S.fresh(), S.fresh()]
                b_acc = [S.fresh() for _ in range(4)]
                b_pT = [S.fresh() for _ in range(4)]
                allb = b_q + b_iq + [b_iw, b_mask, b_e32, b_rs, b_bR, b_bRp, b_blo, b_bnm, b_bcnt, b_bd] + b_maskT + b_acc + b_pT

                for hp in range(2):
                    pk = wpanel(w_in, 0, 16, C_K + hp * 256, 256)
                    for j in range(2):
                        c = hp * 2 + j
                        bk = proj_chunk(pk, j, xnT, b_xn)
                        I("act", "activation", [b_ps[bk]], [b_kT], out=kT[:, c, t0:t0 + T], in_=ps[:, bk, :], func=AF.Copy)
                for hp in range(2):
                    pv = wpanel(w_in, 0, 16, C_V + hp * 256, 256)
                    for tb in range(4):
                        bk = bank()
                        for k in range(16):
                            mm(bk, 256, pv, xnT[:, k, tb * 128:(tb + 1) * 128], pv.v[:, k, :], k == 0, k == 15, [b_xn[k]])
                        I("act", "activation", [b_ps[bk]], [b_V], out=Vtm[:, ti * 4 + tb, hp * 256:(hp + 1) * 256], in_=ps[:, bk, 0:256],
                          func=AF.Copy)
                pik = Panel()
                s_ = rot["slot"]
                rot["slot"] = (s_ + 1) % NSLOT
                slot_gen[s_] += 1
                pik.slot, pik.gen, pik.buf = s_, slot_gen[s_], b_slot[s_]
                pik.v = wslot[s_][:, 0:16 * 144].rearrange("p (k n) -> p k n", n=144)
                ikv = w_in[:, C_IK:C_IK + 64].rearrange("(k p) n -> p k n", p=128)
                iwv = w_in[:, C_IW:C_IW + 16].rearrange("(k p) n -> p k n", p=128)
                DMA("pool", slot_sems[s_], [], [b_slot[s_]], pik.v[:, :, 0:64], ikv)
                DMA("pool", slot_sems[s_], [], [b_slot[s_]], pik.v[:, :, 64:128], ikv)
                DMA("pool", slot_sems[s_], [], [b_slot[s_]], pik.v[:, :, 128:144], iwv)
                bk = proj_chunk(pik, 0, xnT, b_xn)
                I("act", "activation", [b_ps[bk]], [b_ik], out=ikT2[:, t0:t0 + T], in_=ps[:, bk, :], func=AF.Copy)
                for tb in range(4):
                    bk = bank()
                    for k in range(16):
                        mm(bk, 16, pik, xnT[:, k, tb * 128:(tb + 1) * 128], pik.v[:, k, 128:144], k == 0, k == 15, [b_xn[k]])
                    I("act", "activation", [b_ps[bk]], [b_iw], out=iwtm[:, tb, :], in_=ps[:, bk, 0:16], func=AF.Copy, scale=0.03125)
                for hp in range(8):
                    pq = wpanel(w_in, 0, 16, C_Q + hp * 256, 256)
                    for j in range(2):
                        c = hp * 2 + j
                        bk = proj_chunk(pq, j, xnT, b_xn)
                        I("act", "activation", [b_ps[bk]], [b_q[c]], out=qT[:, c, :], in_=ps[:, bk, :], func=AF.Copy,
                          scale=float(128 ** -0.5))
                for hp in range(4):
                    pq = wpanel(w_in, 0, 16, C_IQ + hp * 256, 256)
                    for j in range(2):
                        c = hp * 2 + j
                        bk = proj_chunk(pq, j, xnT, b_xn)
                        I("act", "activation", [b_ps[bk]], [b_iq[c]], out=iqT[:, c, :], in_=ps[:, bk, :], func=AF.Copy)

                def gen_sctk(jj):
                    jb = ti * 4 + jj
                    Sj = (jb + 1) * 128
                    nch = (Sj + 511) // 512
                    mT = maskT[:, jj % 2]
                    bmT = b_maskT[jj % 2]
                    for h in range(16):
                        pb = (h % 2) * 64
                        for cc in range(nch):
                            w = min(512, Sj - cc * 512)
                            bk = bank()
                            I("pe", "matmul", [b_iq[h // 2], b_ik], [b_ps[bk]], ps[:, bk, 0:w],
                              lhsT=iqT[pb:pb + 64, h // 2, jj * 128:(jj + 1) * 128], rhs=ikT2[pb:pb + 64, cc * 512:cc * 512 + w],
                              start=True, stop=True)
                            k2 = (h * nch + cc) % 2
                            I("act", "activation", [b_ps[bk]], [b_sgt[k2]], out=sgt[:, k2, 0:w], in_=ps[:, bk, 0:w], func=AF.Relu)
                            if h == 0:
                                I("dve", "tensor_scalar", [b_sgt[k2], b_iw], [b_acc[cc]], out=acc[:, cc * 512:cc * 512 + w],
                                  in0=sgt[:, k2, 0:w], scalar1=iwtm[:, jj, 0:1], scalar2=None, op0=ALU.mult)
                            else:
                                I("dve", "scalar_tensor_tensor", [b_sgt[k2], b_iw, b_acc[cc]], [b_acc[cc]],
                                  out=acc[:, cc * 512:cc * 512 + w], in0=sgt[:, k2, 0:w], scalar=iwtm[:, jj, h:h + 1],
                                  in1=acc[:, cc * 512:cc * 512 + w], op0=ALU.mult, op1=ALU.add)
                            yield 0.6
                    if jb >= 2:
                        I("dve", "reduce_max", b_acc, [b_bR], out=bis[:, 0:1], in_=acc[:, 0:Sj], axis=AX.X, apply_absolute_value=True)
                        I("dve", "tensor_scalar", [b_bR], [b_bRp], out=bis[:, 1:2], in0=bis[:, 0:1], scalar1=1.0009765625, scalar2=1e-30,
                          op0=ALU.mult, op1=ALU.add)
                        I("dve", "tensor_scalar", [b_bRp], [b_blo], out=bis[:, 2:3], in0=bis[:, 1:2], scalar1=-1.0, scalar2=None,
                          op0=ALU.mult)
                    I("dve", "tensor_tensor", b_acc + [b_causal], b_acc, out=acc[:, jb * 128:(jb + 1) * 128],
                      in0=acc[:, jb * 128:(jb + 1) * 128], in1=causal[:], op=ALU.add)
                    if jb >= 2:
                        for k in range(NPASS):
                            sk = 2.0 ** -k
                            I("dve", "scalar_tensor_tensor", [b_bRp, b_blo], [b_bnm], out=bis[:, 3:4], in0=bis[:, 1:2], scalar=-sk,
                              in1=bis[:, 2:3], op0=ALU.mult, op1=ALU.subtract)
                            I("act", "activation", b_acc + [b_bnm], [b_mask, b_bcnt], out=mask[:, 0:Sj], in_=acc[:, 0:Sj], func=AF.Sign,
                              bias=bis[:, 3:4], scale=1.0, accum_out=bis[:, 4:5])
                            I("dve", "tensor_scalar", [b_bcnt], [b_bd], out=bis[:, 5:6], in0=bis[:, 4:5], scalar1=511.5 - Sj, scalar2=sk,
                              op0=ALU.is_ge, op1=ALU.mult)
                            I("dve", "scalar_tensor_tensor", [b_bd, b_bRp, b_blo], [b_blo], out=bis[:, 2:3], in0=bis[:, 5:6],
                              scalar=bis[:, 1:2], in1=bis[:, 2:3], op0=ALU.mult, op1=ALU.add)
                            yield Sj / 1200.0 + 1.2
                        I("dve", "tensor_scalar", b_acc + [b_blo], [b_mask], out=mask[:, 0:Sj], in0=acc[:, 0:Sj], scalar1=bis[:, 2:3],
                          scalar2=None, op0=ALU.is_ge)
                    else:
                        I("dve", "tensor_scalar", b_acc, [b_mask], out=mask[:, 0:Sj], in0=acc[:, 0:Sj], scalar1=0.5 * NEG,
                          scalar2=None, op0=ALU.is_ge)
                    for i0 in range(0, jb + 1, 8):
                        n = min(8, jb + 1 - i0)
                        for i in range(n):
                            I("pe", "transpose", [b_mask, b_const], [b_psb], out=psb[:, i * 128:(i + 1) * 128],
                              in_=mask[:, (i0 + i) * 128:(i0 + i + 1) * 128], identity=ident_b[:])
                        I("act", "activation", [b_psb, b_const], [bmT], out=mT[:, i0:i0 + n, :],
                          in_=psb[:, 0:n * 128].rearrange("p (i t) -> p i t", i=n), func=AF.Identity, scale=30000.0, bias=c30k[:, 0:1])
                    yield 1.0

                def cost_sctk(jj):
                    jb = ti * 4 + jj
                    Sj = (jb + 1) * 128
                    nch = (Sj + 511) // 512
                    return 16 * nch * 0.6 + (NPASS * (Sj / 1200.0 + 1.2) if jb >= 2 else 0.0) + 1.0

                def gen_att(jj):
                    jb = ti * 4 + jj
                    mT = maskT[:, jj % 2]
                    bmT = b_maskT[jj % 2]
                    DEPTH = 2
                    pend = []

                    def qk(g, i):
                        dl = jb - i
                        bk = bank()
                        I("pe", "matmul", [b_kT] + b_q[4 * g:4 * g + 4], [b_ps[bk]],
                          ps[:, bk, :].rearrange("p (h t) -> p h t", h=4), lhsT=kT[:, g, i * 128:(i + 1) * 128],
                          rhs=qT[:, 4 * g:4 * g + 4, jj * 128:(jj + 1) * 128], start=True, stop=False)
                        for hh in range(4):
                            I("pe", "matmul", [bmT, b_const], [b_ps[bk]], ps[:, bk, hh * 128:(hh + 1) * 128], lhsT=ident_b[:],
                              rhs=mT[:, i, :], start=False, stop=(hh == 3))
                        k2 = pcnt[0] % 4
                        pcnt[0] += 1
                        if dl >= 2:
                            I("act", "activation", [b_ps[bk]], [b_pT[k2]], out=pT[:, k2, :], in_=ps[:, bk, :], func=AF.Exp)
                        else:
                            I("act", "activation", [b_ps[bk]], [b_e32], out=e32[:], in_=ps[:, bk, :], func=AF.Exp)
                            I("dve", "tensor_tensor", [b_e32, b_EB], [b_pT[k2]], out=pT[:, k2, :].rearrange("p (h t) -> p h t", h=4),
                              in0=e32[:].rearrange("p (h t) -> p h t", h=4), in1=EB[:, dl, 4 * g:4 * g + 4, :], op=ALU.mult)
                        return k2

                    def flush_one():
                        g, i, k2 = pend.pop(0)
                        I("pe", "matmul", [b_pT[k2], b_V], [b_ps[5]], ps[:, 5, :], lhsT=Vtm[:, i, g * 128:(g + 1) * 128],
                          rhs=pT[:, k2, :], start=(i == 0), stop=(i == jb))
                        I("pe", "matmul", [b_pT[k2], b_const], [b_ps[6]], ps[:, 6, :], lhsT=ones_b[:], rhs=pT[:, k2, :],
                          start=(i == 0), stop=(i == jb))
                        if i == jb:
                            I("dve", "reciprocal", [b_ps[6]], [b_rs], out=rs[:], in_=ps[:, 6, :])
                            I("dve", "tensor_tensor", [b_ps[5], b_rs], b_ya[4 * g:4 * g + 4],
                              out=yattnT[:, 4 * g:4 * g + 4, jj * 128:(jj + 1) * 128],
                              in0=ps[:, 5, :].rearrange("p (h t) -> p h t", h=4), in1=rs[:].rearrange("p (h t) -> p h t", h=4),
                              op=ALU.mult)

                    for g in range(4):
                        for i in range(jb + 1):
                            pend.append((g, i, qk(g, i)))
                            if len(pend) > DEPTH:
                                flush_one()
                            yield 0.9
                    while pend:
                        flush_one()
                    yield 1.2

                def cost_att(jj):
                    jb = ti * 4 + jj
                    return 4 * (jb + 1) * 0.9 + 1.2

                def interleave(ga, ta, gb, tb):
                    da = db = 0.0
                    la, lb = ga is not None, gb is not None
                    while la or lb:
                        if la and (not lb or da / ta <= db / tb):
                            try:
                                da += next(ga)
                            except StopIteration:
                                la = False
                        else:
                            try:
                                db += next(gb)
                            except StopIteration:
                                lb = False

                interleave(gen_sctk(0), cost_sctk(0), None, 1.0)
                for jj in range(1, 4):
                    interleave(gen_sctk(jj), cost_sctk(jj), gen_att(jj - 1), cost_att(jj - 1))
                interleave(None, 1.0, gen_att(3), cost_att(3))
                S.retire(allb)
            if dbg and ti == 0:
                dump(1, yattnT, b_ya, True)

            yrnnT = es2.enter_context(nc.sbuf_tensor(uq("yrnnT"), [128, 16, T], BF16))
            b_yr = [S.fresh() for _ in range(16)]
            with ExitStack() as ea:
                def sa(name, shape, dt):
                    return ea.enter_context(nc.sbuf_tensor(uq(name), list(shape), dt))
                rxp = sa("rxp", [128, 2, T + 3], F32)
                xc = sa("xc", [128, 2, T], F32)
                xcb = sa("xcb", [128, 2, T], BF16)
                rr = sa("rr", [128, 2, T], F32)
                ig = sa("ig", [128, 2, T], F32)
                aa = sa("aa", [128, 2, T], F32)
                a2 = sa("a2", [128, 2, T], F32)
                uu = sa("uu", [128, 2, T], F32)
                h2 = sa("h2", [128, 2, T], F32)
                gl = sa("gl", [128, 2, T], F32)
                b_rxp = [S.fresh(), S.fresh()]
                b_xc = [S.fresh(), S.fresh()]
                b_xcb = [S.fresh(), S.fresh()]
                b_rr, b_ig, b_aa, b_a2, b_uu = ([S.fresh(), S.fresh()] for _ in range(5))
                b_h2 = [S.fresh(), S.fresh()]
                b_gl = [S.fresh(), S.fresh()]
                allb = b_rxp + b_xc + b_xcb + b_rr + b_ig + b_aa + b_a2 + b_uu + b_h2 + b_gl
                for p in range(8):
                    pw = Panel()
                    s_ = rot["slot"]
                    rot["slot"] = (s_ + 1) % NSLOT
                    slot_gen[s_] += 1
                    pw.slot, pw.gen, pw.buf = s_, slot_gen[s_], b_slot[s_]
                    pw.v = wslot[s_][:, 0:512].rearrange("p (k n) -> p k n", n=128)
                    DMA("pool", slot_sems[s_], [], [b_slot[s_]], pw.v[:, 0:2, :], rgwa[2 * p:2 * p + 2].rearrange("n c d -> c n d"))
                    DMA("pool", slot_sems[s_], [], [b_slot[s_]], pw.v[:, 2:4, :], rgwx[2 * p:2 * p + 2].rearrange("n c d -> c n d"))
                    prx = wpanel(w_in, 0, 16, C_RX + p * 256, 256)
                    prg = wpanel(w_in, 0, 16, C_RG + p * 256, 256)
                    for j in range(2):
                        bk = proj_chunk(prx, j, xnT, b_xn)
                        I("act", "activation", [b_ps[bk]], [b_rxp[j]], out=rxp[:, j, 3:T + 3], in_=ps[:, bk, :], func=AF.Copy)
                    for j in range(2):
                        bk = proj_chunk(prg, j, xnT, b_xn)
                        I("act", "activation", [b_ps[bk]], [b_gl[j]], out=gl[:, j, :], in_=ps[:, bk, :], func=AF.Gelu_apprx_tanh)
                    for j in range(2):
                        c = p * 2 + j
                        I("dve", "tensor_copy", [b_halo], [b_rxp[j]], out=rxp[:, j, 0:3], in_=rxhalo[:, c, :])
                        I("dve", "tensor_copy", [b_rxp[j]], [b_halo], out=rxhalo[:, c, :], in_=rxp[:, j, T:T + 3])
                        I("dve", "tensor_scalar", [b_rxp[j], b_vecs], [b_xc[j]], out=xc[:, j, :], in0=rxp[:, j, 0:T],
                          scalar1=vecs[:, 4, c:c + 1], scalar2=vecs[:, 8, c:c + 1], op0=ALU.mult, op1=ALU.add)
                        for k in range(1, 4):
                            I("dve", "scalar_tensor_tensor", [b_rxp[j], b_vecs, b_xc[j]], [b_xc[j]], out=xc[:, j, :],
                              in0=rxp[:, j, k:k + T], scalar=vecs[:, 4 + k, c:c + 1], in1=xc[:, j, :], op0=ALU.mult, op1=ALU.add)
                        I("dve", "tensor_copy", [b_xc[j]], [b_xcb[j]], out=xcb[:, j, :], in_=xc[:, j, :])
                    gbk = []
                    for j in range(2):
                        bkr, bki = bank(), bank()
                        gbk.append((bkr, bki))
                        mm(bkr, T, pw, pw.v[:, j, :], xcb[:, j, :], True, True, [b_xcb[j]])
                        mm(bki, T, pw, pw.v[:, 2 + j, :], xcb[:, j, :], True, True, [b_xcb[j]])
                    for j in range(2):
                        c = p * 2 + j
                        bkr, bki = gbk[j]
                        I("act", "activation", [b_ps[bkr], b_vecs], [b_rr[j]], out=rr[:, j, :], in_=ps[:, bkr, :], func=AF.Sigmoid,
                          bias=vecs[:, 9, c:c + 1])
                        I("act", "activation", [b_ps[bki], b_vecs], [b_ig[j]], out=ig[:, j, :], in_=ps[:, bki, :], func=AF.Sigmoid,
                          bias=vecs[:, 10, c:c + 1])
                    for j in range(2):
                        c = p * 2 + j
                        I("act", "activation", [b_rr[j], b_nsp], [b_aa[j]], out=aa[:, j, :], in_=rr[:, j, :], func=AF.Exp, scale=nsp8[:, c:c + 1])
                        I("act", "activation", [b_rr[j], b_nsp], [b_a2[j]], out=a2[:, j, :], in_=rr[:, j, :], func=AF.Exp, scale=nsp16[:, c:c + 1])
                    for j in range(2):
                        I("act", "activation", [b_a2[j], b_const], [b_a2[j]], out=a2[:, j, :], in_=a2[:, j, :], func=AF.Sqrt, scale=-1.0,
                          bias=ones_f[:, 0:1])
                    for j in range(2):
                        c = p * 2 + j
                        I("dve", "tensor_tensor", [b_ig[j], b_xc[j]], [b_uu[j]], out=uu[:, j, :], in0=ig[:, j, :], in1=xc[:, j, :], op=ALU.mult)
                        I("dve", "tensor_tensor", [b_uu[j], b_a2[j]], [b_uu[j]], out=uu[:, j, :], in0=uu[:, j, :], in1=a2[:, j, :], op=ALU.mult)
                        I("dve", "tensor_tensor_scan", [b_aa[j], b_uu[j], b_hcar], [b_h2[j]], out=h2[:, j, :], data0=aa[:, j, :], data1=uu[:, j, :],
                          initial=hcar[:, c:c + 1], op0=ALU.mult, op1=ALU.add)
                        I("dve", "tensor_copy", [b_h2[j]], [b_hcar], out=hcar[:, c:c + 1], in_=h2[:, j, T - 1:T])
                    for j in range(2):
                        c = p * 2 + j
                        I("dve", "tensor_tensor", [b_gl[j], b_h2[j]], [b_yr[c]], out=yrnnT[:, c, :], in0=gl[:, j, :], in1=h2[:, j, :],
                          op=ALU.mult)
                S.retire(allb)
            if dbg and ti == 0:
                dump(2, yrnnT, b_yr, True)

            with ExitStack() as ea:
                def sa(name, shape, dt):
                    return ea.enter_context(nc.sbuf_tensor(uq(name), list(shape), dt))
                merged = sa("merged", [128, 16, T], BF16)
                sgr = sa("sgr", [128, 2, T], F32)
                sga = sa("sga", [128, 2, T], F32)
                tm1 = sa("tm1", [128, T], F32)
                tm2 = sa("tm2", [128, T], F32)
                b_mg = [S.fresh() for _ in range(16)]
                b_sgr = [S.fresh(), S.fresh()]
                b_sga = [S.fresh(), S.fresh()]
                b_tm1, b_tm2 = S.fresh(), S.fresh()
                allb = b_mg + b_sgr + b_sga + [b_tm1, b_tm2]
                for p in range(8):
                    pgr = wpanel(w_in, 0, 16, C_GR + p * 256, 256)
                    for j in range(2):
                        bk = proj_chunk(pgr, j, xnT, b_xn)
                        I("act", "activation", [b_ps[bk]], [b_sgr[j]], out=sgr[:, j, :], in_=ps[:, bk, :], func=AF.Sigmoid)
                    pga = wpanel(w_in, 0, 16, C_GA + p * 256, 256)
                    for j in range(2):
                        bk = proj_chunk(pga, j, xnT, b_xn)
                        I("act", "activation", [b_ps[bk]], [b_sga[j]], out=sga[:, j, :], in_=ps[:, bk, :], func=AF.Sigmoid)
                    ppr = wpanel(wpr, 0, 16, p * 256, 256)
                    ppa = wpanel(wpa, 0, 16, p * 256, 256)
                    for j in range(2):
                        c = p * 2 + j
                        b1 = proj_chunk(ppr, j, yrnnT, b_yr)
                        b2 = proj_chunk(ppa, j, yattnT, b_ya)
                        I("dve", "tensor_tensor", [b_ps[b1], b_sgr[j]], [b_tm1], out=tm1[:], in0=sgr[:, j, :], in1=ps[:, b1, :], op=ALU.mult)
                        I("dve", "tensor_tensor", [b_ps[b2], b_sga[j]], [b_tm2], out=tm2[:], in0=sga[:, j, :], in1=ps[:, b2, :], op=ALU.mult)
                        I("dve", "tensor_tensor", [b_tm1, b_tm2], [b_mg[c]], out=merged[:, c, :], in0=tm1[:], in1=tm2[:], op=ALU.add)
                for p in range(8):
                    po = wpanel(wout, 0, 16, p * 256, 256)
                    for j in range(2):
                        c = p * 2 + j
                        bk = proj_chunk(po, j, merged, b_mg)
                        I("dve", "tensor_tensor", [b_ps[bk], b_xT[c]], [b_xT[c]], out=xT[:, c, :], in0=xT[:, c, :], in1=ps[:, bk, :],
                          op=ALU.add)
                S.retire(allb)
            S.retire(b_ya + b_yr)
            es2.close()

            rmsnorm(2, False)
            ffn(w2g, w2u, w2d)
            rmsnorm(3, True)

            with nc.sbuf_tensor(uq("otm"), [128, 2, D], F32) as otm:
                b_otm = [S.fresh(), S.fresh()]
                for tb in range(4):
                    k2 = tb % 2
                    for cg in range(4):
                        bk = bank()
                        for j in range(4):
                            c = cg * 4 + j
                            I("pe", "transpose", [b_xT[c], b_const], [b_ps[bk]], out=ps[:, bk, j * 128:(j + 1) * 128],
                              in_=xT[:, c, tb * 128:(tb + 1) * 128], identity=ident_f[:])
                        I("act", "activation", [b_ps[bk]], [b_otm[k2]], out=otm[:, k2, cg * 512:(cg + 1) * 512], in_=ps[:, bk, :],
                          func=AF.Copy)
                    ob = Buf()
                    out_bufs.append(ob)
                    DMA("sp", d_out[k2], [b_otm[k2]], [ob], out_d[t0 + tb * 128:t0 + (tb + 1) * 128, :], otm[:, k2, :])
                S.retire(b_otm)

        I("sp", "nop", out_bufs, [])
        S.emit(block, esems)
    return nc


_BUCKET = t5_bucket_table()


def _prep_inputs(inp):
    f32 = np.float32

    def fm(v):
        return np.ascontiguousarray(np.asarray(v, f32).reshape(16, 128).T)

    vec_list = [inp["ffn1_norm"][0], inp["mix_norm"][0], inp["ffn2_norm"][0], inp["final_norm"],
                inp["conv_w"][0][0], inp["conv_w"][0][1], inp["conv_w"][0][2], inp["conv_w"][0][3],
                inp["conv_b"][0], inp["rg_b_a"][0], inp["rg_b_x"][0], inp["rg_lambda"][0]]
    vecs = np.ascontiguousarray(np.stack([fm(v) for v in vec_list], axis=1))
    rb = np.asarray(inp["rel_bias"], f32)
    biasT = np.ascontiguousarray(np.transpose(rb[_BUCKET], (1, 0, 3, 2)))
    cvec = np.ascontiguousarray(np.broadcast_to(rb[31][None, :], (128, 16)))
    s = np.arange(128)
    causal = np.where(s[None, :] <= s[:, None], 0.0, NEG).astype(f32)
    shared = {
        "ffn1_w_gate": np.asarray(inp["ffn1_w_gate"][0], f32), "ffn1_w_up": np.asarray(inp["ffn1_w_up"][0], f32),
        "ffn1_w_down": np.asarray(inp["ffn1_w_down"][0], f32),
        "ffn2_w_gate": np.asarray(inp["ffn2_w_gate"][0], f32), "ffn2_w_up": np.asarray(inp["ffn2_w_up"][0], f32),
        "ffn2_w_down": np.asarray(inp["ffn2_w_down"][0], f32),
        "w_in": np.asarray(inp["w_in"][0], f32), "rg_w_a": np.asarray(inp["rg_w_a"][0], f32), "rg_w_x": np.asarray(inp["rg_w_x"][0], f32),
        "w_proj_rnn": np.asarray(inp["w_proj_rnn"][0], f32), "w_proj_attn": np.asarray(inp["w_proj_attn"][0], f32),
        "w_out": np.asarray(inp["w_out"][0], f32),
        "vecs": vecs, "biasT": biasT.astype(f32), "cvec": cvec.astype(f32), "ident": np.eye(128, dtype=f32), "causal": causal,
    }
    return shared


def kernel(**inputs):
    ntiles = int(os.environ.get("KNT", NT))
    dbg = bool(int(os.environ.get("KDBG", "0")))
    shared = _prep_inputs(inputs)
    x = np.asarray(inputs["x"], np.float32)
    nc = build(ntiles, dbg)
    in_maps = []
    for b in range(8):
        m = dict(shared)
        m["x"] = np.ascontiguousarray(x[b])
        in_maps.append(m)
    res = run_bass_kernel_spmd(nc, in_maps, core_ids=list(range(8)))
    out = np.stack([np.asarray(r["out"], np.float32) for r in res.results], axis=0)
    if dbg:
        kernel.dbg = res.results[0]
    return out
```

```python
import os
import numpy as np
import concourse.bass as bass
import concourse.mybir as mybir
from concourse.bass_utils import run_bass_kernel_spmd

F32 = mybir.dt.float32
BF16 = mybir.dt.bfloat16
AF = mybir.ActivationFunctionType
ALU = mybir.AluOpType
AX = mybir.AxisListType
NPASS = 28

D = 2048
SEQ = 2048
T = 512
NT = SEQ // T
DFF = 5504
NFC = DFF // 128
NIN = 12368
C_RX, C_RG, C_Q, C_K, C_V, C_IQ, C_IK, C_IW, C_GR, C_GA = 0, 2048, 4096, 6144, 6656, 7168, 8192, 8256, 8272, 10320
NEG = -1.0e30
ENGS = ("pe", "act", "dve", "pool", "sp")


class Buf:
    __slots__ = ("writer", "readers", "dreaders")

    def __init__(self, pending=None):
        self.writer = None
        self.readers = dict(pending[0]) if pending else {}
        self.dreaders = list(pending[1]) if pending else []


class Inst:
    __slots__ = ("eng", "fn", "deps", "signal", "sigval", "dma_sem", "dma_val", "idx")

    def __init__(self, eng, fn):
        self.eng = eng
        self.fn = fn
        self.deps = []
        self.signal = False
        self.sigval = 0
        self.dma_sem = None
        self.dma_val = 0


class Sched:
    def __init__(self):
        self.q = {e: [] for e in ENGS}
        self.dma_counts = {}
        self.n = 0
        self.pending = ({}, [])

    def fresh(self):
        return Buf(self.pending)

    def retire(self, bufs):
        pr, pd = self.pending
        for b in bufs:
            cands = list(b.readers.values())
            if b.writer is not None:
                cands.append(b.writer)
            cands += b.dreaders
            for c in cands:
                if c.dma_sem is not None:
                    pd.append(c)
                else:
                    o = pr.get(c.eng)
                    if o is None or o.idx < c.idx:
                        pr[c.eng] = c
        if len(pd) > 8:
            del pd[:-8]

    def issue(self, eng, fn, reads=(), writes=(), dma_sem=None):
        inst = Inst(eng, fn)
        inst.idx = self.n
        self.n += 1
        deps = {}
        is_dma = dma_sem is not None

        def same(o):
            return (not is_dma) and o.dma_sem is None and o.eng == eng

        def skip(o):
            return same(o) and eng == "pe"

        for b in reads:
            w = b.writer
            if w is not None and not skip(w):
                deps[id(w)] = w
        for b in writes:
            w = b.writer
            if w is not None and not skip(w):
                deps[id(w)] = w
            for r in b.readers.values():
                if not skip(r):
                    deps[id(r)] = r
            for r in b.dreaders:
                deps[id(r)] = r
        inst.deps = list(deps.values())
        for d in inst.deps:
            if d.dma_sem is None:
                d.signal = True
        if is_dma:
            c = self.dma_counts.get(id(dma_sem), 0) + 16
            self.dma_counts[id(dma_sem)] = c
            inst.dma_sem = dma_sem
            inst.dma_val = c
        for b in writes:
            b.writer = inst
            b.readers = {}
            b.dreaders = []
        for b in reads:
            if b.writer is not inst:
                if is_dma:
                    b.dreaders.append(inst)
                else:
                    b.readers[eng] = inst
        self.q[eng].append(inst)
        return inst

    def emit(self, block, esems):
        for e in ENGS:
            c = 0
            for inst in self.q[e]:
                if inst.dma_sem is None and inst.signal:
                    c += 1
                    inst.sigval = c

        def run(ename, engobj):
            seen = {}
            for inst in self.q[ename]:
                for d in inst.deps:
                    if d.dma_sem is not None:
                        sem, val = d.dma_sem, d.dma_val
                    else:
                        sem, val = esems[d.eng], d.sigval
                    k = id(sem)
                    if seen.get(k, 0) >= val:
                        continue
                    seen[k] = val
                    engobj.wait_ge(sem, val)
                bi = inst.fn(engobj)
                if inst.dma_sem is not None:
                    bi.then_inc(inst.dma_sem, 16)
                elif inst.signal:
                    bi.then_inc(esems[ename], 1)

        @block.tensor
        def _(eng):
            run("pe", eng)

        @block.scalar
        def _(eng):
            run("act", eng)

        @block.vector
        def _(eng):
            run("dve", eng)

        @block.gpsimd
        def _(eng):
            run("pool", eng)

        @block.sync
        def _(eng):
            run("sp", eng)


def t5_bucket_table():
    s = np.arange(128, dtype=np.int32)[:, None]
    t = np.arange(128, dtype=np.int32)[None, :]
    out = []
    for delta in (0, 1):
        rel = t - s + 128 * delta
        n = np.maximum(rel, 0)
        nf = np.maximum(n, 1).astype(np.float32)
        large = 16 + (np.log(nf / np.float32(16)) / np.float32(np.log(128 / 16)) * np.float32(16)).astype(np.int32)
        large = np.minimum(large, 31)
        out.append(np.where(n < 16, n, large))
    return np.stack(out, 0)


def build(ntiles=NT, dbg=False):
    nc = bass.Bass("TRN2", target_bir_lowering=False)

    def din(name, shape):
        return nc.dram_tensor(name, list(shape), F32, kind="ExternalInput").ap()

    x_d = din("x", [SEQ, D])
    w1g, w1u, w1d = din("ffn1_w_gate", [D, DFF]), din("ffn1_w_up", [D, DFF]), din("ffn1_w_down", [DFF, D])
    w2g, w2u, w2d = din("ffn2_w_gate", [D, DFF]), din("ffn2_w_up", [D, DFF]), din("ffn2_w_down", [DFF, D])
    w_in = din("w_in", [D, NIN])
    rgwa, rgwx = din("rg_w_a", [16, 128, 128]), din("rg_w_x", [16, 128, 128])
    wpr, wpa, wout = din("w_proj_rnn", [D, D]), din("w_proj_attn", [D, D]), din("w_out", [D, D])
    vecs_d = din("vecs", [128, 12, 16])
    biasT_d = din("biasT", [128, 2, 16, 128])
    cvec_d = din("cvec", [128, 16])
    ident_d = din("ident", [128, 128])
    causal_d = din("causal", [128, 128])
    out_d = nc.dram_tensor("out", [SEQ, D], F32, kind="ExternalOutput").ap()
    if dbg:
        dbg_d = nc.dram_tensor("dbg", [3, 128, 16, T], F32, kind="ExternalOutput").ap()
        dbgb_d = nc.dram_tensor("dbgb", [3, 128, 16, T], BF16, kind="ExternalOutput").ap()

    S = Sched()
    NSLOT = 4
    SLOTW = 256

    from contextlib import ExitStack
    with ExitStack() as es:
        ucnt = [0]

        def uq(name):
            ucnt[0] += 1
            return "%s_u%d" % (name, ucnt[0])

        def sb(name, shape, dt):
            return es.enter_context(nc.sbuf_tensor(uq(name), list(shape), dt))

        def sem(name):
            return es.enter_context(nc.semaphore(name))

        esems = {e: sem("s_" + e) for e in ENGS}
        slot_sems = [sem("d_slot%d" % i) for i in range(NSLOT)]
        d_c = [sem("d_c%d" % i) for i in range(5)]
        d_xin = [sem("d_xin0"), sem("d_xin1")]
        d_out = [sem("d_out0"), sem("d_out1")]
        d_dbg = sem("d_dbg")

        ident_f = sb("ident_f", [128, 128], F32)
        ident_b = sb("ident_b", [128, 128], BF16)
        ones_f = sb("ones_f", [128, 128], F32)
        ones_b = sb("ones_b", [128, 128], BF16)
        causal = sb("causal", [128, 128], F32)
        vecs = sb("vecs", [128, 12, 16], F32)
        nsp8 = sb("nsp8", [128, 16], F32)
        nsp16 = sb("nsp16", [128, 16], F32)
        negc = sb("negc", [128, 16], F32)
        epsT = sb("epsT", [128, 1], F32)
        EB = sb("EB", [128, 2, 16, 128], BF16)
        kT = sb("kT", [128, 4, SEQ], BF16)
        Vtm = sb("Vtm", [128, 16, 512], BF16)
        ikT2 = sb("ikT2", [128, SEQ], BF16)
        hcar = sb("hcar", [128, 16], F32)
        rxhalo = sb("rxhalo", [128, 16, 3], F32)
        xT = sb("xT", [128, 16, T], F32)
        xnT = sb("xnT", [128, 16, T], BF16)
        wslot = [sb("wslot%d" % i, [128, 16 * SLOTW], BF16) for i in range(NSLOT)]
        c30k = sb("c30k", [128, 1], F32)
        sgt = sb("sgt", [128, 2, T], F32)
        ps = es.enter_context(nc.psum_tensor("ps", [128, 7, 512], F32))
        psb = es.enter_context(nc.psum_tensor("psb", [128, 1024], BF16))
        block = es.enter_context(nc.Block())

        B = Buf
        b_const, b_vecs, b_nsp, b_EB, b_causal, b_negc = B(), B(), B(), B(), B(), B()
        b_kT, b_V, b_ik, b_hcar, b_halo = B(), B(), B(), B(), B()
        b_xT = [B() for _ in range(16)]
        b_xn = [B() for _ in range(16)]
        b_slot = [B() for _ in range(NSLOT)]
        b_sgt = [B(), B()]
        b_ps = [B() for _ in range(7)]
        b_psb = B()
        out_bufs = []

        def I(eng, method, reads, writes, *a, **kw):
            return S.issue(eng, lambda e: getattr(e, method)(*a, **kw), reads, writes)

        def DMA(eng, semh, reads, writes, out, in_, **kw):
            return S.issue(eng, lambda e: e.dma_start(out=out, in_=in_, **kw), reads, writes, dma_sem=semh)

        rot = {"bank": 0, "slot": 0}
        slot_gen = [0] * NSLOT

        def bank():
            b = rot["bank"]
            rot["bank"] = (b + 1) % 5
            return b

        class Panel:
            pass

        def load_panel(src3, nk, ncols):
            assert nk * ncols <= 16 * SLOTW
            s = rot["slot"]
            rot["slot"] = (s + 1) % NSLOT
            slot_gen[s] += 1
            p = Panel()
            p.slot, p.gen, p.buf = s, slot_gen[s], b_slot[s]
            p.v = wslot[s][:, 0:nk * ncols].rearrange("p (k n) -> p k n", n=ncols)
            DMA("pool", slot_sems[s], [], [b_slot[s]], p.v, src3)
            return p

        def wpanel(w, k0, nk, c0, ncols):
            return load_panel(w[k0 * 128:(k0 + nk) * 128, c0:c0 + ncols].rearrange("(k p) n -> p k n", p=128), nk, ncols)

        def mm(bk, cols, panel, lhsT, rhs, start, stop, reads):
            if panel is not None:
                assert slot_gen[panel.slot] == panel.gen, "stale weight panel"
                reads = list(reads) + [panel.buf]
            out = ps[:, bk, 0:cols]
            I("pe", "matmul", reads, [b_ps[bk]], out, lhsT=lhsT, rhs=rhs, start=start, stop=stop)

        DMA("sp", d_c[0], [], [b_const], ident_f[:], ident_d)
        DMA("sp", d_c[1], [], [b_causal], causal[:], causal_d)
        DMA("sp", d_c[2], [], [b_vecs], vecs[:], vecs_d)
        DMA("sp", d_c[3], [], [b_negc], negc[:], cvec_d)
        I("dve", "tensor_copy", [b_const], [b_const], out=ident_b[:], in_=ident_f[:])
        I("dve", "memset", [], [b_const], ones_f[:], 1.0)
        I("dve", "memset", [], [b_const], ones_b[:], 1.0)
        I("dve", "memset", [], [b_const], epsT[:], 1.0e-6)
        I("dve", "memset", [], [b_const], c30k[:], -30000.0)
        I("dve", "memset", [], [b_hcar], hcar[:], 0.0)
        I("dve", "memset", [], [b_halo], rxhalo[:], 0.0)
        I("act", "activation", [b_vecs, b_const], [b_nsp], out=nsp8[:], in_=vecs[:, 11, :], func=AF.Exp, scale=-1.0)
        I("act", "activation", [b_nsp, b_const], [b_nsp], out=nsp8[:], in_=nsp8[:], func=AF.Ln, bias=ones_f[:, 0:1])
        I("dve", "tensor_scalar", [b_nsp], [b_nsp], out=nsp16[:], in0=nsp8[:], scalar1=-16.0, scalar2=None, op0=ALU.mult)
        I("dve", "tensor_scalar", [b_nsp], [b_nsp], out=nsp8[:], in0=nsp8[:], scalar1=-8.0, scalar2=None, op0=ALU.mult)
        I("dve", "tensor_scalar", [b_negc], [b_negc], out=negc[:], in0=negc[:], scalar1=-1.0, scalar2=None, op0=ALU.mult)
        with nc.sbuf_tensor(uq("biasT"), [128, 2, 16, 128], F32) as biasT:
            b_bT = S.fresh()
            DMA("sp", d_c[4], [], [b_bT], biasT[:], biasT_d)
            for dl in range(2):
                for h in range(16):
                    I("act", "activation", [b_bT, b_negc], [b_EB], out=EB[:, dl, h, :], in_=biasT[:, dl, h, :], func=AF.Exp,
                      bias=negc[:, h:h + 1])
            S.retire([b_bT])

        def rmsnorm(gidx, out_f32):
            with nc.sbuf_tensor(uq("sq"), [128, 2, T], F32) as sq, nc.sbuf_tensor(uq("rstd"), [128, T], F32) as rstd:
                b_sq = [S.fresh(), S.fresh()]
                b_rstd = S.fresh()
                bk = bank()
                for c in range(16):
                    I("act", "activation", [b_xT[c]], [b_sq[c % 2]], out=sq[:, c % 2, :], in_=xT[:, c, :], func=AF.Square)
                    I("pe", "matmul", [b_sq[c % 2], b_const], [b_ps[bk]], ps[:, bk, :], lhsT=ones_f[:], rhs=sq[:, c % 2, :],
                      start=(c == 0), stop=(c == 15))
                I("act", "activation", [b_ps[bk], b_const], [b_rstd], out=rstd[:], in_=ps[:, bk, :], func=AF.Sqrt, scale=1.0 / D,
                  bias=epsT[:, 0:1])
                I("dve", "reciprocal", [b_rstd], [b_rstd], out=rstd[:], in_=rstd[:])
                for c in range(16):
                    if out_f32:
                        I("dve", "scalar_tensor_tensor", [b_xT[c], b_vecs, b_rstd], [b_xT[c]], out=xT[:, c, :], in0=xT[:, c, :],
                          scalar=vecs[:, gidx, c:c + 1], in1=rstd[:], op0=ALU.mult, op1=ALU.mult)
                    else:
                        I("dve", "scalar_tensor_tensor", [b_xT[c], b_vecs, b_rstd], [b_xn[c]], out=xnT[:, c, :], in0=xT[:, c, :],
                          scalar=vecs[:, gidx, c:c + 1], in1=rstd[:], op0=ALU.mult, op1=ALU.mult)
                S.retire(b_sq + [b_rstd])

        def proj_chunk(panel, j, rhs_tile, rhs_bufs, nk=16, cols=T):
            bk = bank()
            for k in range(nk):
                mm(bk, cols, panel, panel.v[:, k, j * 128:(j + 1) * 128], rhs_tile[:, k, 0:cols], k == 0, k == nk - 1,
                   [rhs_bufs[k]])
            return bk

        def ffn(wg, wu, wd):
            NQ = 11
            with nc.sbuf_tensor(uq("actT"), [128, NQ, T], BF16) as actT:
                b_act = [S.fresh() for _ in range(NQ)]
                for q0 in range(0, NFC, NQ):
                    q1 = min(NFC, q0 + NQ)
                    for p0 in range(q0, q1, 2):
                        n = min(2, q1 - p0)
                        pg = wpanel(wg, 0, 16, p0 * 128, n * 128)
                        pu = wpanel(wu, 0, 16, p0 * 128, n * 128)
                        for j in range(n):
                            fc = p0 + j
                            bg = proj_chunk(pg, j, xnT, b_xn)
                            bu = proj_chunk(pu, j, xnT, b_xn)
                            k2 = fc % 2
                            I("act", "activation", [b_ps[bg]], [b_sgt[k2]], out=sgt[:, k2, :], in_=ps[:, bg, :], func=AF.Silu)
                            I("dve", "tensor_tensor", [b_sgt[k2], b_ps[bu]], [b_act[fc - q0]], out=actT[:, fc - q0, :],
                              in0=sgt[:, k2, :], in1=ps[:, bu, :], op=ALU.mult)
                    nq = q1 - q0
                    for grp in range(8):
                        pd = wpanel(wd, q0, nq, grp * 256, 256)
                        for j in range(2):
                            c = grp * 2 + j
                            bk = bank()
                            for f in range(nq):
                                mm(bk, T, pd, pd.v[:, f, j * 128:(j + 1) * 128], actT[:, f, :], f == 0, f == nq - 1, [b_act[f]])
                            I("dve", "scalar_tensor_tensor", [b_ps[bk], b_xT[c]], [b_xT[c]], out=xT[:, c, :], in0=ps[:, bk, :],
                              scalar=0.5, in1=xT[:, c, :], op0=ALU.mult, op1=ALU.add)
                S.retire(b_act)

        def dump(idx, tile, bufs, bf):
            ob = Buf()
            out_bufs.append(ob)
            DMA("sp", d_dbg, bufs, [ob], (dbgb_d if bf else dbg_d)[idx], tile[:])

        for ti in range(ntiles):
            t0 = ti * T
            with nc.sbuf_tensor(uq("xtm"), [128, 2, D], F32) as xtm:
                b_xtm = [S.fresh(), S.fresh()]
                for tb in range(4):
                    k2 = tb % 2
                    DMA("sp", d_xin[k2], [], [b_xtm[k2]], xtm[:, k2, :], x_d[t0 + tb * 128:t0 + (tb + 1) * 128, :])
                    for cg in range(4):
                        bk = bank()
                        for j in range(4):
                            c = cg * 4 + j
                            I("pe", "transpose", [b_xtm[k2], b_const], [b_ps[bk]], out=ps[:, bk, j * 128:(j + 1) * 128],
                              in_=xtm[:, k2, c * 128:(c + 1) * 128], identity=ident_f[:])
                        I("act", "activation", [b_ps[bk]], b_xT[cg * 4:cg * 4 + 4], out=xT[:, cg * 4:cg * 4 + 4, tb * 128:(tb + 1) * 128],
                          in_=ps[:, bk, :].rearrange("p (c t) -> p c t", c=4), func=AF.Copy)
                S.retire(b_xtm)

            rmsnorm(0, False)
            ffn(w1g, w1u, w1d)
            if dbg and ti == 0:
                dump(0, xT, b_xT, False)

            rmsnorm(1, False)
            es2 = ExitStack()
            yattnT = es2.enter_context(nc.sbuf_tensor(uq("yattnT"), [128, 16, T], BF16))
            b_ya = [S.fresh() for _ in range(16)]

            with ExitStack() as ea:
                def sa(name, shape, dt):
                    return ea.enter_context(nc.sbuf_tensor(uq(name), list(shape), dt))
                qT = sa("qT", [128, 16, T], BF16)
                iqT = sa("iqT", [128, 8, T], BF16)
                iwtm = sa("iwtm", [128, 4, 16], F32)
                acc = sa("acc", [128, SEQ], F32)
                bis = sa("bis", [128, 8], F32)
                mask = sa("mask", [128, SEQ], BF16)
                maskT = sa("maskT", [128, 2, 16, 128], BF16)
                e32 = sa("e32", [128, 512], F32)
                pT = sa("pT", [128, 4, 512], BF16)
                pcnt = [0]
                rs = sa("rs", [128, 512], F32)
                b_q = [S.fresh() for _ in range(16)]
                b_iq = [S.fresh() for _ in range(8)]
                b_iw, b_mask, b_e32, b_rs = (S.fresh() for _ in range(4))
                b_bR, b_bRp, b_blo, b_bnm, b_bcnt, b_bd = (S.fresh() for _ in range(6))
                b_maskT = [S.fresh(), S.fresh()]
                b_acc = [S.fresh() for _ in range(4)]
                b_pT = [S.fresh() for _ in range(4)]
                allb = b_q + b_iq + [b_iw, b_mask, b_e32, b_rs, b_bR, b_bRp, b_blo, b_bnm, b_bcnt, b_bd] + b_maskT + b_acc + b_pT

                for hp in range(2):
                    pk = wpanel(w_in, 0, 16, C_K + hp * 256, 256)
                    for j in range(2):
                        c = hp * 2 + j
                        bk = proj_chunk(pk, j, xnT, b_xn)
                        I("act", "activation", [b_ps[bk]], [b_kT], out=kT[:, c, t0:t0 + T], in_=ps[:, bk, :], func=AF.Copy)
                for hp in range(2):
                    pv = wpanel(w_in, 0, 16, C_V + hp * 256, 256)
                    for tb in range(4):
                        bk = bank()
                        for k in range(16):
                            mm(bk, 256, pv, xnT[:, k, tb * 128:(tb + 1) * 128], pv.v[:, k, :], k == 0, k == 15, [b_xn[k]])
                        I("act", "activation", [b_ps[bk]], [b_V], out=Vtm[:, ti * 4 + tb, hp * 256:(hp + 1) * 256], in_=ps[:, bk, 0:256],
                          func=AF.Copy)
                pik = Panel()
                s_ = rot["slot"]
                rot["slot"] = (s_ + 1) % NSLOT
                slot_gen[s_] += 1
                pik.slot, pik.gen, pik.buf = s_, slot_gen[s_], b_slot[s_]
                pik.v = wslot[s_][:, 0:16 * 144].rearrange("p (k n) -> p k n", n=144)
                ikv = w_in[:, C_IK:C_IK + 64].rearrange("(k p) n -> p k n", p=128)
                iwv = w_in[:, C_IW:C_IW + 16].rearrange("(k p) n -> p k n", p=128)
                DMA("pool", slot_sems[s_], [], [b_slot[s_]], pik.v[:, :, 0:64], ikv)
                DMA("pool", slot_sems[s_], [], [b_slot[s_]], pik.v[:, :, 64:128], ikv)
                DMA("pool", slot_sems[s_], [], [b_slot[s_]], pik.v[:, :, 128:144], iwv)
                bk = proj_chunk(pik, 0, xnT, b_xn)
                I("act", "activation", [b_ps[bk]], [b_ik], out=ikT2[:, t0:t0 + T], in_=ps[:, bk, :], func=AF.Copy)
                for tb in range(4):
                    bk = bank()
                    for k in range(16):
                        mm(bk, 16, pik, xnT[:, k, tb * 128:(tb + 1) * 128], pik.v[:, k, 128:144], k == 0, k == 15, [b_xn[k]])
                    I("act", "activation", [b_ps[bk]], [b_iw], out=iwtm[:, tb, :], in_=ps[:, bk, 0:16], func=AF.Copy, scale=0.03125)
                for hp in range(4):
                    pq = wpanel(w_in, 0, 16, C_IQ + hp * 256, 256)
                    for j in range(2):
                        c = hp * 2 + j
                        bk = proj_chunk(pq, j, xnT, b_xn)
                        I("act", "activation", [b_ps[bk]], [b_iq[c]], out=iqT[:, c, :], in_=ps[:, bk, :], func=AF.Copy)

                def gen_qproj():
                    for hp in range(8):
                        pq = wpanel(w_in, 0, 16, C_Q + hp * 256, 256)
                        for j in range(2):
                            c = hp * 2 + j
                            bk = proj_chunk(pq, j, xnT, b_xn)
                            I("act", "activation", [b_ps[bk]], [b_q[c]], out=qT[:, c, :], in_=ps[:, bk, :], func=AF.Copy,
                              scale=float(128 ** -0.5))
                            yield 4.5

                def gen_sctk(jj):
                    jb = ti * 4 + jj
                    Sj = (jb + 1) * 128
                    nch = (Sj + 511) // 512
                    mT = maskT[:, jj % 2]
                    bmT = b_maskT[jj % 2]
                    for h in range(16):
                        pb = (h % 2) * 64
                        for cc in range(nch):
                            w = min(512, Sj - cc * 512)
                            bk = bank()
                            I("pe", "matmul", [b_iq[h // 2], b_ik], [b_ps[bk]], ps[:, bk, 0:w],
                              lhsT=iqT[pb:pb + 64, h // 2, jj * 128:(jj + 1) * 128], rhs=ikT2[pb:pb + 64, cc * 512:cc * 512 + w],
                              start=True, stop=True)
                            k2 = (h * nch + cc) % 2
                            I("act", "activation", [b_ps[bk]], [b_sgt[k2]], out=sgt[:, k2, 0:w], in_=ps[:, bk, 0:w], func=AF.Relu)
                            if h == 0:
                                I("dve", "tensor_scalar", [b_sgt[k2], b_iw], [b_acc[cc]], out=acc[:, cc * 512:cc * 512 + w],
                                  in0=sgt[:, k2, 0:w], scalar1=iwtm[:, jj, 0:1], scalar2=None, op0=ALU.mult)
                            else:
                                I("dve", "scalar_tensor_tensor", [b_sgt[k2], b_iw, b_acc[cc]], [b_acc[cc]],
                                  out=acc[:, cc * 512:cc * 512 + w], in0=sgt[:, k2, 0:w], scalar=iwtm[:, jj, h:h + 1],
                                  in1=acc[:, cc * 512:cc * 512 + w], op0=ALU.mult, op1=ALU.add)
                            yield 0.6
                    if jb >= 2:
                        I("dve", "reduce_max", b_acc, [b_bR], out=bis[:, 0:1], in_=acc[:, 0:Sj], axis=AX.X, apply_absolute_value=True)
                        I("dve", "tensor_scalar", [b_bR], [b_bRp], out=bis[:, 1:2], in0=bis[:, 0:1], scalar1=1.0009765625, scalar2=1e-30,
                          op0=ALU.mult, op1=ALU.add)
                        I("dve", "tensor_scalar", [b_bRp], [b_blo], out=bis[:, 2:3], in0=bis[:, 1:2], scalar1=-1.0, scalar2=None,
                          op0=ALU.mult)
                    I("dve", "tensor_tensor", b_acc + [b_causal], b_acc, out=acc[:, jb * 128:(jb + 1) * 128],
                      in0=acc[:, jb * 128:(jb + 1) * 128], in1=causal[:], op=ALU.add)
                    if jb >= 2:
                        for k in range(NPASS):
                            sk = 2.0 ** -k
                            I("dve", "scalar_tensor_tensor", [b_bRp, b_blo], [b_bnm], out=bis[:, 3:4], in0=bis[:, 1:2], scalar=-sk,
                              in1=bis[:, 2:3], op0=ALU.mult, op1=ALU.subtract)
                            I("act", "activation", b_acc + [b_bnm], [b_mask, b_bcnt], out=mask[:, 0:Sj], in_=acc[:, 0:Sj], func=AF.Sign,
                              bias=bis[:, 3:4], scale=1.0, accum_out=bis[:, 4:5])
                            I("dve", "tensor_scalar", [b_bcnt], [b_bd], out=bis[:, 5:6], in0=bis[:, 4:5], scalar1=511.5 - Sj, scalar2=sk,
                              op0=ALU.is_ge, op1=ALU.mult)
                            I("dve", "scalar_tensor_tensor", [b_bd, b_bRp, b_blo], [b_blo], out=bis[:, 2:3], in0=bis[:, 5:6],
                              scalar=bis[:, 1:2], in1=bis[:, 2:3], op0=ALU.mult, op1=ALU.add)
                            yield Sj / 1200.0 + 1.2
                        I("dve", "tensor_scalar", b_acc + [b_blo], [b_mask], out=mask[:, 0:Sj], in0=acc[:, 0:Sj], scalar1=bis[:, 2:3],
                          scalar2=None, op0=ALU.is_ge)
                    else:
                        I("dve", "tensor_scalar", b_acc, [b_mask], out=mask[:, 0:Sj], in0=acc[:, 0:Sj], scalar1=0.5 * NEG,
                          scalar2=None, op0=ALU.is_ge)
                    for i0 in range(0, jb + 1, 8):
                        n = min(8, jb + 1 - i0)
                        for i in range(n):
                            I("pe", "transpose", [b_mask, b_const], [b_psb], out=psb[:, i * 128:(i + 1) * 128],
                              in_=mask[:, (i0 + i) * 128:(i0 + i + 1) * 128], identity=ident_b[:])
                        I("act", "activation", [b_psb, b_const], [bmT], out=mT[:, i0:i0 + n, :],
                          in_=psb[:, 0:n * 128].rearrange("p (i t) -> p i t", i=n), func=AF.Identity, scale=30000.0, bias=c30k[:, 0:1])
                    yield 1.0

                def cost_sctk(jj):
                    jb = ti * 4 + jj
                    Sj = (jb + 1) * 128
                    nch = (Sj + 511) // 512
                    return 16 * nch * 0.6 + (NPASS * (Sj / 1200.0 + 1.2) if jb >= 2 else 0.0) + 1.0

                def gen_att(jj):
                    jb = ti * 4 + jj
                    mT = maskT[:, jj % 2]
                    bmT = b_maskT[jj % 2]
                    DEPTH = 2
                    pend = []

                    def qk(g, i):
                        dl = jb - i
                        bk = bank()
                        I("pe", "matmul", [b_kT] + b_q[4 * g:4 * g + 4], [b_ps[bk]],
                          ps[:, bk, :].rearrange("p (h t) -> p h t", h=4), lhsT=kT[:, g, i * 128:(i + 1) * 128],
                          rhs=qT[:, 4 * g:4 * g + 4, jj * 128:(jj + 1) * 128], start=True, stop=False)
                        for hh in range(4):
                            I("pe", "matmul", [bmT, b_const], [b_ps[bk]], ps[:, bk, hh * 128:(hh + 1) * 128], lhsT=ident_b[:],
                              rhs=mT[:, i, :], start=False, stop=(hh == 3))
                        k2 = pcnt[0] % 4
                        pcnt[0] += 1
                        if dl >= 2:
                            I("act", "activation", [b_ps[bk]], [b_pT[k2]], out=pT[:, k2, :], in_=ps[:, bk, :], func=AF.Exp)
                        else:
                            I("act", "activation", [b_ps[bk]], [b_e32], out=e32[:], in_=ps[:, bk, :], func=AF.Exp)
                            I("dve", "tensor_tensor", [b_e32, b_EB], [b_pT[k2]], out=pT[:, k2, :].rearrange("p (h t) -> p h t", h=4),
                              in0=e32[:].rearrange("p (h t) -> p h t", h=4), in1=EB[:, dl, 4 * g:4 * g + 4, :], op=ALU.mult)
                        return k2

                    def flush_one():
                        g, i, k2 = pend.pop(0)
                        I("pe", "matmul", [b_pT[k2], b_V], [b_ps[5]], ps[:, 5, :], lhsT=Vtm[:, i, g * 128:(g + 1) * 128],
                          rhs=pT[:, k2, :], start=(i == 0), stop=(i == jb))
                        I("pe", "matmul", [b_pT[k2], b_const], [b_ps[6]], ps[:, 6, :], lhsT=ones_b[:], rhs=pT[:, k2, :],
                          start=(i == 0), stop=(i == jb))
                        if i == jb:
                            I("dve", "reciprocal", [b_ps[6]], [b_rs], out=rs[:], in_=ps[:, 6, :])
                            I("dve", "tensor_tensor", [b_ps[5], b_rs], b_ya[4 * g:4 * g + 4],
                              out=yattnT[:, 4 * g:4 * g + 4, jj * 128:(jj + 1) * 128],
                              in0=ps[:, 5, :].rearrange("p (h t) -> p h t", h=4), in1=rs[:].rearrange("p (h t) -> p h t", h=4),
                              op=ALU.mult)

                    for g in range(4):
                        for i in range(jb + 1):
                            pend.append((g, i, qk(g, i)))
                            if len(pend) > DEPTH:
                                flush_one()
                            yield 0.9
                    while pend:
                        flush_one()
                    yield 1.2

                def cost_att(jj):
                    jb = ti * 4 + jj
                    return 4 * (jb + 1) * 0.9 + 1.2

                def interleave(ga, ta, gb, tb):
                    da = db = 0.0
                    la, lb = ga is not None, gb is not None
                    while la or lb:
                        if la and (not lb or da / ta <= db / tb):
                            try:
                                da += next(ga)
                            except StopIteration:
                                la = False
                        else:
                            try:
                                db += next(gb)
                            except StopIteration:
                                lb = False

                interleave(gen_sctk(0), cost_sctk(0), gen_qproj(), 72.0)
                for jj in range(1, 4):
                    interleave(gen_sctk(jj), cost_sctk(jj), gen_att(jj - 1), cost_att(jj - 1))
                interleave(None, 1.0, gen_att(3), cost_att(3))
                S.retire(allb)
            if dbg and ti == 0:
                dump(1, yattnT, b_ya, True)

            yrnnT = es2.enter_context(nc.sbuf_tensor(uq("yrnnT"), [128, 16, T], BF16))
            b_yr = [S.fresh() for _ in range(16)]
            with ExitStack() as ea:
                def sa(name, shape, dt):
                    return ea.enter_context(nc.sbuf_tensor(uq(name), list(shape), dt))
                rxp = sa("rxp", [128, 2, T + 3], F32)
                xc = sa("xc", [128, 2, T], F32)
                xcb = sa("xcb", [128, 2, T], BF16)
                rr = sa("rr", [128, 2, T], F32)
                ig = sa("ig", [128, 2, T], F32)
                aa = sa("aa", [128, 2, T], F32)
                a2 = sa("a2", [128, 2, T], F32)
                uu = sa("uu", [128, 2, T], F32)
                h2 = sa("h2", [128, 2, T], F32)
                gl = sa("gl", [128, 2, T], F32)
                b_rxp = [S.fresh(), S.fresh()]
                b_xc = [S.fresh(), S.fresh()]
                b_xcb = [S.fresh(), S.fresh()]
                b_rr, b_ig, b_aa, b_a2, b_uu = ([S.fresh(), S.fresh()] for _ in range(5))
                b_h2 = [S.fresh(), S.fresh()]
                b_gl = [S.fresh(), S.fresh()]
                allb = b_rxp + b_xc + b_xcb + b_rr + b_ig + b_aa + b_a2 + b_uu + b_h2 + b_gl
                for p in range(8):
                    pw = Panel()
                    s_ = rot["slot"]
                    rot["slot"] = (s_ + 1) % NSLOT
                    slot_gen[s_] += 1
                    pw.slot, pw.gen, pw.buf = s_, slot_gen[s_], b_slot[s_]
                    pw.v = wslot[s_][:, 0:512].rearrange("p (k n) -> p k n", n=128)
                    DMA("pool", slot_sems[s_], [], [b_slot[s_]], pw.v[:, 0:2, :], rgwa[2 * p:2 * p + 2].rearrange("n c d -> c n d"))
                    DMA("pool", slot_sems[s_], [], [b_slot[s_]], pw.v[:, 2:4, :], rgwx[2 * p:2 * p + 2].rearrange("n c d -> c n d"))
                    prx = wpanel(w_in, 0, 16, C_RX + p * 256, 256)
                    prg = wpanel(w_in, 0, 16, C_RG + p * 256, 256)
                    for j in range(2):
                        bk = proj_chunk(prx, j, xnT, b_xn)
                        I("act", "activation", [b_ps[bk]], [b_rxp[j]], out=rxp[:, j, 3:T + 3], in_=ps[:, bk, :], func=AF.Copy)
                    for j in range(2):
                        bk = proj_chunk(prg, j, xnT, b_xn)
                        I("act", "activation", [b_ps[bk]], [b_gl[j]], out=gl[:, j, :], in_=ps[:, bk, :], func=AF.Gelu_apprx_tanh)
                    for j in range(2):
                        c = p * 2 + j
                        I("dve", "tensor_copy", [b_halo], [b_rxp[j]], out=rxp[:, j, 0:3], in_=rxhalo[:, c, :])
                        I("dve", "tensor_copy", [b_rxp[j]], [b_halo], out=rxhalo[:, c, :], in_=rxp[:, j, T:T + 3])
                        I("dve", "tensor_scalar", [b_rxp[j], b_vecs], [b_xc[j]], out=xc[:, j, :], in0=rxp[:, j, 0:T],
                          scalar1=vecs[:, 4, c:c + 1], scalar2=vecs[:, 8, c:c + 1], op0=ALU.mult, op1=ALU.add)
                        for k in range(1, 4):
                            I("dve", "scalar_tensor_tensor", [b_rxp[j], b_vecs, b_xc[j]], [b_xc[j]], out=xc[:, j, :],
                              in0=rxp[:, j, k:k + T], scalar=vecs[:, 4 + k, c:c + 1], in1=xc[:, j, :], op0=ALU.mult, op1=ALU.add)
                        I("dve", "tensor_copy", [b_xc[j]], [b_xcb[j]], out=xcb[:, j, :], in_=xc[:, j, :])
                    gbk = []
                    for j in range(2):
                        bkr, bki = bank(), bank()
                        gbk.append((bkr, bki))
                        mm(bkr, T, pw, pw.v[:, j, :], xcb[:, j, :], True, True, [b_xcb[j]])
                        mm(bki, T, pw, pw.v[:, 2 + j, :], xcb[:, j, :], True, True, [b_xcb[j]])
                    for j in range(2):
                        c = p * 2 + j
                        bkr, bki = gbk[j]
                        I("act", "activation", [b_ps[bkr], b_vecs], [b_rr[j]], out=rr[:, j, :], in_=ps[:, bkr, :], func=AF.Sigmoid,
                          bias=vecs[:, 9, c:c + 1])
                        I("act", "activation", [b_ps[bki], b_vecs], [b_ig[j]], out=ig[:, j, :], in_=ps[:, bki, :], func=AF.Sigmoid,
                          bias=vecs[:, 10, c:c + 1])
                    for j in range(2):
                        c = p * 2 + j
                        I("act", "activation", [b_rr[j], b_nsp], [b_aa[j]], out=aa[:, j, :], in_=rr[:, j, :], func=AF.Exp, scale=nsp8[:, c:c + 1])
                        I("act", "activation", [b_rr[j], b_nsp], [b_a2[j]], out=a2[:, j, :], in_=rr[:, j, :], func=AF.Exp, scale=nsp16[:, c:c + 1])
                    for j in range(2):
                        I("act", "activation", [b_a2[j], b_const], [b_a2[j]], out=a2[:, j, :], in_=a2[:, j, :], func=AF.Sqrt, scale=-1.0,
                          bias=ones_f[:, 0:1])
                    for j in range(2):
                        c = p * 2 + j
                        I("dve", "tensor_tensor", [b_ig[j], b_xc[j]], [b_uu[j]], out=uu[:, j, :], in0=ig[:, j, :], in1=xc[:, j, :], op=ALU.mult)
                        I("dve", "tensor_tensor", [b_uu[j], b_a2[j]], [b_uu[j]], out=uu[:, j, :], in0=uu[:, j, :], in1=a2[:, j, :], op=ALU.mult)
                        I("dve", "tensor_tensor_scan", [b_aa[j], b_uu[j], b_hcar], [b_h2[j]], out=h2[:, j, :], data0=aa[:, j, :], data1=uu[:, j, :],
                          initial=hcar[:, c:c + 1], op0=ALU.mult, op1=ALU.add)
                        I("dve", "tensor_copy", [b_h2[j]], [b_hcar], out=hcar[:, c:c + 1], in_=h2[:, j, T - 1:T])
                    for j in range(2):
                        c = p * 2 + j
                        I("dve", "tensor_tensor", [b_gl[j], b_h2[j]], [b_yr[c]], out=yrnnT[:, c, :], in0=gl[:, j, :], in1=h2[:, j, :],
                          op=ALU.mult)
                S.retire(allb)
            if dbg and ti == 0:
                dump(2, yrnnT, b_yr, True)

            with ExitStack() as ea:
                def sa(name, shape, dt):
                    return ea.enter_context(nc.sbuf_tensor(uq(name), list(shape), dt))
                merged = sa("merged", [128, 16, T], BF16)
                sgr = sa("sgr", [128, 2, T], F32)
                sga = sa("sga", [128, 2, T], F32)
                tm1 = sa("tm1", [128, T], F32)
                tm2 = sa("tm2", [128, T], F32)
                b_mg = [S.fresh() for _ in range(16)]
                b_sgr = [S.fresh(), S.fresh()]
                b_sga = [S.fresh(), S.fresh()]
                b_tm1, b_tm2 = S.fresh(), S.fresh()
                allb = b_mg + b_sgr + b_sga + [b_tm1, b_tm2]
                for p in range(8):
                    pgr = wpanel(w_in, 0, 16, C_GR + p * 256, 256)
                    for j in range(2):
                        bk = proj_chunk(pgr, j, xnT, b_xn)
                        I("act", "activation", [b_ps[bk]], [b_sgr[j]], out=sgr[:, j, :], in_=ps[:, bk, :], func=AF.Sigmoid)
                    pga = wpanel(w_in, 0, 16, C_GA + p * 256, 256)
                    for j in range(2):
                        bk = proj_chunk(pga, j, xnT, b_xn)
                        I("act", "activation", [b_ps[bk]], [b_sga[j]], out=sga[:, j, :], in_=ps[:, bk, :], func=AF.Sigmoid)
                    ppr = wpanel(wpr, 0, 16, p * 256, 256)
                    ppa = wpanel(wpa, 0, 16, p * 256, 256)
                    for j in range(2):
                        c = p * 2 + j
                        b1 = proj_chunk(ppr, j, yrnnT, b_yr)
                        b2 = proj_chunk(ppa, j, yattnT, b_ya)
                        I("dve", "tensor_tensor", [b_ps[b1], b_sgr[j]], [b_tm1], out=tm1[:], in0=sgr[:, j, :], in1=ps[:, b1, :], op=ALU.mult)
                        I("dve", "tensor_tensor", [b_ps[b2], b_sga[j]], [b_tm2], out=tm2[:], in0=sga[:, j, :], in1=ps[:, b2, :], op=ALU.mult)
                        I("dve", "tensor_tensor", [b_tm1, b_tm2], [b_mg[c]], out=merged[:, c, :], in0=tm1[:], in1=tm2[:], op=ALU.add)
                for p in range(8):
                    po = wpanel(wout, 0, 16, p * 256, 256)
                    for j in range(2):
                        c = p * 2 + j
                        bk = proj_chunk(po, j, merged, b_mg)
                        I("dve", "tensor_tensor", [b_ps[bk], b_xT[c]], [b_xT[c]], out=xT[:, c, :], in0=xT[:, c, :], in1=ps[:, bk, :],
                          op=ALU.add)
                S.retire(allb)
            S.retire(b_ya + b_yr)
            es2.close()

            rmsnorm(2, False)
            ffn(w2g, w2u, w2d)
            rmsnorm(3, True)

            with nc.sbuf_tensor(uq("otm"), [128, 2, D], F32) as otm:
                b_otm = [S.fresh(), S.fresh()]
                for tb in range(4):
                    k2 = tb % 2
                    for cg in range(4):
                        bk = bank()
                        for j in range(4):
                            c = cg * 4 + j
                            I("pe", "transpose", [b_xT[c], b_const], [b_ps[bk]], out=ps[:, bk, j * 128:(j + 1) * 128],
                              in_=xT[:, c, tb * 128:(tb + 1) * 128], identity=ident_f[:])
                        I("act", "activation", [b_ps[bk]], [b_otm[k2]], out=otm[:, k2, cg * 512:(cg + 1) * 512], in_=ps[:, bk, :],
                          func=AF.Copy)
                    ob = Buf()
                    out_bufs.append(ob)
                    DMA("sp", d_out[k2], [b_otm[k2]], [ob], out_d[t0 + tb * 128:t0 + (tb + 1) * 128, :], otm[:, k2, :])
                S.retire(b_otm)

        I("sp", "nop", out_bufs, [])
        S.emit(block, esems)
    return nc


_BUCKET = t5_bucket_table()


def _prep_inputs(inp):
    f32 = np.float32

    def fm(v):
        return np.ascontiguousarray(np.asarray(v, f32).reshape(16, 128).T)

    vec_list = [inp["ffn1_norm"][0], inp["mix_norm"][0], inp["ffn2_norm"][0], inp["final_norm"],
                inp["conv_w"][0][0], inp["conv_w"][0][1], inp["conv_w"][0][2], inp["conv_w"][0][3],
                inp["conv_b"][0], inp["rg_b_a"][0], inp["rg_b_x"][0], inp["rg_lambda"][0]]
    vecs = np.ascontiguousarray(np.stack([fm(v) for v in vec_list], axis=1))
    rb = np.asarray(inp["rel_bias"], f32)
    biasT = np.ascontiguousarray(np.transpose(rb[_BUCKET], (1, 0, 3, 2)))
    cvec = np.ascontiguousarray(np.broadcast_to(rb[31][None, :], (128, 16)))
    s = np.arange(128)
    causal = np.where(s[None, :] <= s[:, None], 0.0, NEG).astype(f32)
    shared = {
        "ffn1_w_gate": np.asarray(inp["ffn1_w_gate"][0], f32), "ffn1_w_up": np.asarray(inp["ffn1_w_up"][0], f32),
        "ffn1_w_down": np.asarray(inp["ffn1_w_down"][0], f32),
        "ffn2_w_gate": np.asarray(inp["ffn2_w_gate"][0], f32), "ffn2_w_up": np.asarray(inp["ffn2_w_up"][0], f32),
        "ffn2_w_down": np.asarray(inp["ffn2_w_down"][0], f32),
        "w_in": np.asarray(inp["w_in"][0], f32), "rg_w_a": np.asarray(inp["rg_w_a"][0], f32), "rg_w_x": np.asarray(inp["rg_w_x"][0], f32),
        "w_proj_rnn": np.asarray(inp["w_proj_rnn"][0], f32), "w_proj_attn": np.asarray(inp["w_proj_attn"][0], f32),
        "w_out": np.asarray(inp["w_out"][0], f32),
        "vecs": vecs, "biasT": biasT.astype(f32), "cvec": cvec.astype(f32), "ident": np.eye(128, dtype=f32), "causal": causal,
    }
    return shared


def kernel(**inputs):
    ntiles = int(os.environ.get("KNT", NT))
    dbg = bool(int(os.environ.get("KDBG", "0")))
    shared = _prep_inputs(inputs)
    x = np.asarray(inputs["x"], np.float32)
    nc = build(ntiles, dbg)
    in_maps = []
    for b in range(8):
        m = dict(shared)
        m["x"] = np.ascontiguousarray(x[b])
        in_maps.append(m)
    res = run_bass_kernel_spmd(nc, in_maps, core_ids=list(range(8)))
    out = np.stack([np.asarray(r["out"], np.float32) for r in res.results], axis=0)
    if dbg:
        kernel.dbg = res.results[0]
    return out
```

```python
import os
import numpy as np
import concourse.bass as bass
import concourse.mybir as mybir
from concourse.bass_utils import run_bass_kernel_spmd

F32 = mybir.dt.float32
BF16 = mybir.dt.bfloat16
AF = mybir.ActivationFunctionType
ALU = mybir.AluOpType
AX = mybir.AxisListType
NPASS = 28

D = 2048
SEQ = 2048
T = 512
NT = SEQ // T
DFF = 5504
NFC = DFF // 128
NIN = 12368
C_RX, C_RG, C_Q, C_K, C_V, C_IQ, C_IK, C_IW, C_GR, C_GA = 0, 2048, 4096, 6144, 6656, 7168, 8192, 8256, 8272, 10320
NEG = -1.0e30
ENGS = ("pe", "act", "dve", "pool", "sp")


class Buf:
    __slots__ = ("writer", "readers", "dreaders")

    def __init__(self, pending=None):
        self.writer = None
        self.readers = dict(pending[0]) if pending else {}
        self.dreaders = list(pending[1]) if pending else []


class Inst:
    __slots__ = ("eng", "fn", "deps", "signal", "sigval", "dma_sem", "dma_val", "idx")

    def __init__(self, eng, fn):
        self.eng = eng
        self.fn = fn
        self.deps = []
        self.signal = False
        self.sigval = 0
        self.dma_sem = None
        self.dma_val = 0


class Sched:
    def __init__(self):
        self.q = {e: [] for e in ENGS}
        self.dma_counts = {}
        self.n = 0
        self.pending = ({}, [])

    def fresh(self):
        return Buf(self.pending)

    def retire(self, bufs):
        pr, pd = self.pending
        for b in bufs:
            cands = list(b.readers.values())
            if b.writer is not None:
                cands.append(b.writer)
            cands += b.dreaders
            for c in cands:
                if c.dma_sem is not None:
                    pd.append(c)
                else:
                    o = pr.get(c.eng)
                    if o is None or o.idx < c.idx:
                        pr[c.eng] = c
        if len(pd) > 8:
            del pd[:-8]

    def issue(self, eng, fn, reads=(), writes=(), dma_sem=None):
        inst = Inst(eng, fn)
        inst.idx = self.n
        self.n += 1
        deps = {}
        is_dma = dma_sem is not None

        def same(o):
            return (not is_dma) and o.dma_sem is None and o.eng == eng

        def skip(o):
            return same(o) and eng == "pe"

        for b in reads:
            w = b.writer
            if w is not None and not skip(w):
                deps[id(w)] = w
        for b in writes:
            w = b.writer
            if w is not None and not skip(w):
                deps[id(w)] = w
            for r in b.readers.values():
                if not skip(r):
                    deps[id(r)] = r
            for r in b.dreaders:
                deps[id(r)] = r
        inst.deps = list(deps.values())
        for d in inst.deps:
            if d.dma_sem is None:
                d.signal = True
        if is_dma:
            c = self.dma_counts.get(id(dma_sem), 0) + 16
            self.dma_counts[id(dma_sem)] = c
            inst.dma_sem = dma_sem
            inst.dma_val = c
        for b in writes:
            b.writer = inst
            b.readers = {}
            b.dreaders = []
        for b in reads:
            if b.writer is not inst:
                if is_dma:
                    b.dreaders.append(inst)
                else:
                    b.readers[eng] = inst
        self.q[eng].append(inst)
        return inst

    def emit(self, block, esems):
        for e in ENGS:
            c = 0
            for inst in self.q[e]:
                if inst.dma_sem is None and inst.signal:
                    c += 1
                    inst.sigval = c

        def run(ename, engobj):
            seen = {}
            for inst in self.q[ename]:
                for d in inst.deps:
                    if d.dma_sem is not None:
                        sem, val = d.dma_sem, d.dma_val
                    else:
                        sem, val = esems[d.eng], d.sigval
                    k = id(sem)
                    if seen.get(k, 0) >= val:
                        continue
                    seen[k] = val
                    engobj.wait_ge(sem, val)
                bi = inst.fn(engobj)
                if inst.dma_sem is not None:
                    bi.then_inc(inst.dma_sem, 16)
                elif inst.signal:
                    bi.then_inc(esems[ename], 1)

        @block.tensor
        def _(eng):
            run("pe", eng)

        @block.scalar
        def _(eng):
            run("act", eng)

        @block.vector
        def _(eng):
            run("dve", eng)

        @block.gpsimd
        def _(eng):
            run("pool", eng)

        @block.sync
        def _(eng):
            run("sp", eng)


def t5_bucket_table():
    s = np.arange(128, dtype=np.int32)[:, None]
    t = np.arange(128, dtype=np.int32)[None, :]
    out = []
    for delta in (0, 1):
        rel = t - s + 128 * delta
        n = np.maximum(rel, 0)
        nf = np.maximum(n, 1).astype(np.float32)
        large = 16 + (np.log(nf / np.float32(16)) / np.float32(np.log(128 / 16)) * np.float32(16)).astype(np.int32)
        large = np.minimum(large, 31)
        out.append(np.where(n < 16, n, large))
    return np.stack(out, 0)


def build(ntiles=NT, dbg=False):
    nc = bass.Bass("TRN2", target_bir_lowering=False)

    def din(name, shape):
        return nc.dram_tensor(name, list(shape), F32, kind="ExternalInput").ap()

    x_d = din("x", [SEQ, D])
    w1g, w1u, w1d = din("ffn1_w_gate", [D, DFF]), din("ffn1_w_up", [D, DFF]), din("ffn1_w_down", [DFF, D])
    w2g, w2u, w2d = din("ffn2_w_gate", [D, DFF]), din("ffn2_w_up", [D, DFF]), din("ffn2_w_down", [DFF, D])
    w_in = din("w_in", [D, NIN])
    rgwa, rgwx = din("rg_w_a", [16, 128, 128]), din("rg_w_x", [16, 128, 128])
    wpr, wpa, wout = din("w_proj_rnn", [D, D]), din("w_proj_attn", [D, D]), din("w_out", [D, D])
    vecs_d = din("vecs", [128, 12, 16])
    biasT_d = din("biasT", [128, 2, 16, 128])
    cvec_d = din("cvec", [128, 16])
    ident_d = din("ident", [128, 128])
    causal_d = din("causal", [128, 128])
    out_d = nc.dram_tensor("out", [SEQ, D], F32, kind="ExternalOutput").ap()
    if dbg:
        dbg_d = nc.dram_tensor("dbg", [3, 128, 16, T], F32, kind="ExternalOutput").ap()
        dbgb_d = nc.dram_tensor("dbgb", [3, 128, 16, T], BF16, kind="ExternalOutput").ap()

    S = Sched()
    NSLOT = 4
    SLOTW = 256

    from contextlib import ExitStack
    with ExitStack() as es:
        ucnt = [0]

        def uq(name):
            ucnt[0] += 1
            return "%s_u%d" % (name, ucnt[0])

        def sb(name, shape, dt):
            return es.enter_context(nc.sbuf_tensor(uq(name), list(shape), dt))

        def sem(name):
            return es.enter_context(nc.semaphore(name))

        esems = {e: sem("s_" + e) for e in ENGS}
        slot_sems = [sem("d_slot%d" % i) for i in range(NSLOT)]
        d_c = [sem("d_c%d" % i) for i in range(5)]
        d_xin = [sem("d_xin0"), sem("d_xin1")]
        d_out = [sem("d_out0"), sem("d_out1")]
        d_dbg = sem("d_dbg")

        ident_f = sb("ident_f", [128, 128], F32)
        ident_b = sb("ident_b", [128, 128], BF16)
        ones_f = sb("ones_f", [128, 128], F32)
        ones_b = sb("ones_b", [128, 128], BF16)
        causal = sb("causal", [128, 128], F32)
        vecs = sb("vecs", [128, 12, 16], F32)
        nsp8 = sb("nsp8", [128, 16], F32)
        nsp16 = sb("nsp16", [128, 16], F32)
        negc = sb("negc", [128, 16], F32)
        epsT = sb("epsT", [128, 1], F32)
        EB = sb("EB", [128, 2, 16, 128], BF16)
        kT = sb("kT", [128, 4, SEQ], BF16)
        Vtm = sb("Vtm", [128, 16, 512], BF16)
        ikT2 = sb("ikT2", [128, SEQ], BF16)
        hcar = sb("hcar", [128, 16], F32)
        rxhalo = sb("rxhalo", [128, 16, 3], F32)
        xT = sb("xT", [128, 16, T], F32)
        xnT = sb("xnT", [128, 16, T], BF16)
        wslot = [sb("wslot%d" % i, [128, 16 * SLOTW], BF16) for i in range(NSLOT)]
        c30k = sb("c30k", [128, 1], F32)
        sgt = sb("sgt", [128, 2, T], F32)
        ps = es.enter_context(nc.psum_tensor("ps", [128, 7, 512], F32))
        psb = es.enter_context(nc.psum_tensor("psb", [128, 1024], BF16))
        block = es.enter_context(nc.Block())

        B = Buf
        b_const, b_vecs, b_nsp, b_EB, b_causal, b_negc = B(), B(), B(), B(), B(), B()
        b_kT, b_V, b_ik, b_hcar, b_halo = B(), B(), B(), B(), B()
        b_xT = [B() for _ in range(16)]
        b_xn = [B() for _ in range(16)]
        b_slot = [B() for _ in range(NSLOT)]
        b_sgt = [B(), B()]
        b_ps = [B() for _ in range(7)]
        b_psb = B()
        out_bufs = []

        def I(eng, method, reads, writes, *a, **kw):
            return S.issue(eng, lambda e: getattr(e, method)(*a, **kw), reads, writes)

        def DMA(eng, semh, reads, writes, out, in_, **kw):
            return S.issue(eng, lambda e: e.dma_start(out=out, in_=in_, **kw), reads, writes, dma_sem=semh)

        rot = {"bank": 0, "slot": 0}
        slot_gen = [0] * NSLOT

        def bank():
            b = rot["bank"]
            rot["bank"] = (b + 1) % 5
            return b

        class Panel:
            pass

        def load_panel(src3, nk, ncols):
            assert nk * ncols <= 16 * SLOTW
            s = rot["slot"]
            rot["slot"] = (s + 1) % NSLOT
            slot_gen[s] += 1
            p = Panel()
            p.slot, p.gen, p.buf = s, slot_gen[s], b_slot[s]
            p.v = wslot[s][:, 0:nk * ncols].rearrange("p (k n) -> p k n", n=ncols)
            DMA("pool", slot_sems[s], [], [b_slot[s]], p.v, src3)
            return p

        def wpanel(w, k0, nk, c0, ncols):
            return load_panel(w[k0 * 128:(k0 + nk) * 128, c0:c0 + ncols].rearrange("(k p) n -> p k n", p=128), nk, ncols)

        def mm(bk, cols, panel, lhsT, rhs, start, stop, reads):
            if panel is not None:
                assert slot_gen[panel.slot] == panel.gen, "stale weight panel"
                reads = list(reads) + [panel.buf]
            out = ps[:, bk, 0:cols]
            I("pe", "matmul", reads, [b_ps[bk]], out, lhsT=lhsT, rhs=rhs, start=start, stop=stop)

        DMA("sp", d_c[0], [], [b_const], ident_f[:], ident_d)
        DMA("sp", d_c[1], [], [b_causal], causal[:], causal_d)
        DMA("sp", d_c[2], [], [b_vecs], vecs[:], vecs_d)
        DMA("sp", d_c[3], [], [b_negc], negc[:], cvec_d)
        I("dve", "tensor_copy", [b_const], [b_const], out=ident_b[:], in_=ident_f[:])
        I("dve", "memset", [], [b_const], ones_f[:], 1.0)
        I("dve", "memset", [], [b_const], ones_b[:], 1.0)
        I("dve", "memset", [], [b_const], epsT[:], 1.0e-6)
        I("dve", "memset", [], [b_const], c30k[:], -30000.0)
        I("dve", "memset", [], [b_hcar], hcar[:], 0.0)
        I("dve", "memset", [], [b_halo], rxhalo[:], 0.0)
        I("act", "activation", [b_vecs, b_const], [b_nsp], out=nsp8[:], in_=vecs[:, 11, :], func=AF.Exp, scale=-1.0)
        I("act", "activation", [b_nsp, b_const], [b_nsp], out=nsp8[:], in_=nsp8[:], func=AF.Ln, bias=ones_f[:, 0:1])
        I("dve", "tensor_scalar", [b_nsp], [b_nsp], out=nsp16[:], in0=nsp8[:], scalar1=-16.0, scalar2=None, op0=ALU.mult)
        I("dve", "tensor_scalar", [b_nsp], [b_nsp], out=nsp8[:], in0=nsp8[:], scalar1=-8.0, scalar2=None, op0=ALU.mult)
        I("dve", "tensor_scalar", [b_negc], [b_negc], out=negc[:], in0=negc[:], scalar1=-1.0, scalar2=None, op0=ALU.mult)
        with nc.sbuf_tensor(uq("biasT"), [128, 2, 16, 128], F32) as biasT:
            b_bT = S.fresh()
            DMA("sp", d_c[4], [], [b_bT], biasT[:], biasT_d)
            for dl in range(2):
                for h in range(16):
                    I("act", "activation", [b_bT, b_negc], [b_EB], out=EB[:, dl, h, :], in_=biasT[:, dl, h, :], func=AF.Exp,
                      bias=negc[:, h:h + 1])
            S.retire([b_bT])

        def rmsnorm(gidx, out_f32):
            with nc.sbuf_tensor(uq("sq"), [128, 2, T], F32) as sq, nc.sbuf_tensor(uq("rstd"), [128, T], F32) as rstd:
                b_sq = [S.fresh(), S.fresh()]
                b_rstd = S.fresh()
                bk = bank()
                for c in range(16):
                    I("act", "activation", [b_xT[c]], [b_sq[c % 2]], out=sq[:, c % 2, :], in_=xT[:, c, :], func=AF.Square)
                    if c == 1:
                        I("dve", "tensor_tensor", b_sq, [b_rstd], out=rstd[:], in0=sq[:, 0, :], in1=sq[:, 1, :], op=ALU.add)
                    elif c > 1:
                        I("dve", "tensor_tensor", [b_sq[c % 2], b_rstd], [b_rstd], out=rstd[:], in0=sq[:, c % 2, :], in1=rstd[:], op=ALU.add)
                I("pe", "matmul", [b_rstd, b_const], [b_ps[bk]], ps[:, bk, :], lhsT=ones_f[:], rhs=rstd[:], start=True, stop=True)
                I("act", "activation", [b_ps[bk], b_const], [b_rstd], out=rstd[:], in_=ps[:, bk, :], func=AF.Sqrt, scale=1.0 / D,
                  bias=epsT[:, 0:1])
                I("dve", "reciprocal", [b_rstd], [b_rstd], out=rstd[:], in_=rstd[:])
                for c in range(16):
                    if out_f32:
                        I("dve", "scalar_tensor_tensor", [b_xT[c], b_vecs, b_rstd], [b_xT[c]], out=xT[:, c, :], in0=xT[:, c, :],
                          scalar=vecs[:, gidx, c:c + 1], in1=rstd[:], op0=ALU.mult, op1=ALU.mult)
                    else:
                        I("dve", "scalar_tensor_tensor", [b_xT[c], b_vecs, b_rstd], [b_xn[c]], out=xnT[:, c, :], in0=xT[:, c, :],
                          scalar=vecs[:, gidx, c:c + 1], in1=rstd[:], op0=ALU.mult, op1=ALU.mult)
                S.retire(b_sq + [b_rstd])

        def proj_chunk(panel, j, rhs_tile, rhs_bufs, nk=16, cols=T):
            bk = bank()
            for k in range(nk):
                mm(bk, cols, panel, panel.v[:, k, j * 128:(j + 1) * 128], rhs_tile[:, k, 0:cols], k == 0, k == nk - 1,
                   [rhs_bufs[k]])
            return bk

        def ffn(wg, wu, wd):
            NQ = 11
            with nc.sbuf_tensor(uq("actT"), [128, NQ, T], BF16) as actT:
                b_act = [S.fresh() for _ in range(NQ)]
                for q0 in range(0, NFC, NQ):
                    q1 = min(NFC, q0 + NQ)
                    for p0 in range(q0, q1, 2):
                        n = min(2, q1 - p0)
                        pg = wpanel(wg, 0, 16, p0 * 128, n * 128)
                        pu = wpanel(wu, 0, 16, p0 * 128, n * 128)
                        for j in range(n):
                            fc = p0 + j
                            bg = proj_chunk(pg, j, xnT, b_xn)
                            bu = proj_chunk(pu, j, xnT, b_xn)
                            k2 = fc % 2
                            I("act", "activation", [b_ps[bg]], [b_sgt[k2]], out=sgt[:, k2, :], in_=ps[:, bg, :], func=AF.Silu)
                            I("dve", "tensor_tensor", [b_sgt[k2], b_ps[bu]], [b_act[fc - q0]], out=actT[:, fc - q0, :],
                              in0=sgt[:, k2, :], in1=ps[:, bu, :], op=ALU.mult)
                    nq = q1 - q0
                    for grp in range(8):
                        pd = wpanel(wd, q0, nq, grp * 256, 256)
                        for j in range(2):
                            c = grp * 2 + j
                            bk = bank()
                            for f in range(nq):
                                mm(bk, T, pd, pd.v[:, f, j * 128:(j + 1) * 128], actT[:, f, :], f == 0, f == nq - 1, [b_act[f]])
                            I("dve", "scalar_tensor_tensor", [b_ps[bk], b_xT[c]], [b_xT[c]], out=xT[:, c, :], in0=ps[:, bk, :],
                              scalar=0.5, in1=xT[:, c, :], op0=ALU.mult, op1=ALU.add)
                S.retire(b_act)

        def dump(idx, tile, bufs, bf):
            ob = Buf()
            out_bufs.append(ob)
            DMA("sp", d_dbg, bufs, [ob], (dbgb_d if bf else dbg_d)[idx], tile[:])

        for ti in range(ntiles):
            t0 = ti * T
            with nc.sbuf_tensor(uq("xtm"), [128, 2, D], F32) as xtm:
                b_xtm = [S.fresh(), S.fresh()]
                for tb in range(4):
                    k2 = tb % 2
                    DMA("sp", d_xin[k2], [], [b_xtm[k2]], xtm[:, k2, :], x_d[t0 + tb * 128:t0 + (tb + 1) * 128, :])
                    for cg in range(4):
                        bk = bank()
                        for j in range(4):
                            c = cg * 4 + j
                            I("pe", "transpose", [b_xtm[k2], b_const], [b_ps[bk]], out=ps[:, bk, j * 128:(j + 1) * 128],
                              in_=xtm[:, k2, c * 128:(c + 1) * 128], identity=ident_f[:])
                        I("act", "activation", [b_ps[bk]], b_xT[cg * 4:cg * 4 + 4], out=xT[:, cg * 4:cg * 4 + 4, tb * 128:(tb + 1) * 128],
                          in_=ps[:, bk, :].rearrange("p (c t) -> p c t", c=4), func=AF.Copy)
                S.retire(b_xtm)

            rmsnorm(0, False)
            ffn(w1g, w1u, w1d)
            if dbg and ti == 0:
                dump(0, xT, b_xT, False)

            rmsnorm(1, False)
            es2 = ExitStack()
            yattnT = es2.enter_context(nc.sbuf_tensor(uq("yattnT"), [128, 16, T], BF16))
            b_ya = [S.fresh() for _ in range(16)]

            with ExitStack() as ea:
                def sa(name, shape, dt):
                    return ea.enter_context(nc.sbuf_tensor(uq(name), list(shape), dt))
                qT = sa("qT", [128, 16, T], BF16)
                iqT = sa("iqT", [128, 8, T], BF16)
                iwtm = sa("iwtm", [128, 4, 16], F32)
                acc = sa("acc", [128, SEQ], F32)
                bis = sa("bis", [128, 8], F32)
                mask = sa("mask", [128, SEQ], BF16)
                maskT = sa("maskT", [128, 2, 16, 128], BF16)
                e32 = sa("e32", [128, 512], F32)
                pT = sa("pT", [128, 4, 512], BF16)
                pcnt = [0]
                rs = sa("rs", [128, 512], F32)
                b_q = [S.fresh() for _ in range(16)]
                b_iq = [S.fresh() for _ in range(8)]
                b_iw, b_mask, b_e32, b_rs = (S.fresh() for _ in range(4))
                b_bR, b_bRp, b_blo, b_bnm, b_bcnt, b_bd = (S.fresh() for _ in range(6))
                b_maskT = [S.fresh(), S.fresh()]
                b_acc = [S.fresh() for _ in range(4)]
                b_pT = [S.fresh() for _ in range(4)]
                allb = b_q + b_iq + [b_iw, b_mask, b_e32, b_rs, b_bR, b_bRp, b_blo, b_bnm, b_bcnt, b_bd] + b_maskT + b_acc + b_pT

                for hp in range(2):
                    pk = wpanel(w_in, 0, 16, C_K + hp * 256, 256)
                    for j in range(2):
                        c = hp * 2 + j
                        bk = proj_chunk(pk, j, xnT, b_xn)
                        I("act", "activation", [b_ps[bk]], [b_kT], out=kT[:, c, t0:t0 + T], in_=ps[:, bk, :], func=AF.Copy)
                for hp in range(2):
                    pv = wpanel(w_in, 0, 16, C_V + hp * 256, 256)
                    for tb in range(4):
                        bk = bank()
                        for k in range(16):
                            mm(bk, 256, pv, xnT[:, k, tb * 128:(tb + 1) * 128], pv.v[:, k, :], k == 0, k == 15, [b_xn[k]])
                        I("act", "activation", [b_ps[bk]], [b_V], out=Vtm[:, ti * 4 + tb, hp * 256:(hp + 1) * 256], in_=ps[:, bk, 0:256],
                          func=AF.Copy)
                pik = Panel()
                s_ = rot["slot"]
                rot["slot"] = (s_ + 1) % NSLOT
                slot_gen[s_] += 1
                pik.slot, pik.gen, pik.buf = s_, slot_gen[s_], b_slot[s_]
                pik.v = wslot[s_][:, 0:16 * 144].rearrange("p (k n) -> p k n", n=144)
                ikv = w_in[:, C_IK:C_IK + 64].rearrange("(k p) n -> p k n", p=128)
                iwv = w_in[:, C_IW:C_IW + 16].rearrange("(k p) n -> p k n", p=128)
                DMA("pool", slot_sems[s_], [], [b_slot[s_]], pik.v[:, :, 0:64], ikv)
                DMA("pool", slot_sems[s_], [], [b_slot[s_]], pik.v[:, :, 64:128], ikv)
                DMA("pool", slot_sems[s_], [], [b_slot[s_]], pik.v[:, :, 128:144], iwv)
                bk = proj_chunk(pik, 0, xnT, b_xn)
                I("act", "activation", [b_ps[bk]], [b_ik], out=ikT2[:, t0:t0 + T], in_=ps[:, bk, :], func=AF.Copy)
                for tb in range(4):
                    bk = bank()
                    for k in range(16):
                        mm(bk, 16, pik, xnT[:, k, tb * 128:(tb + 1) * 128], pik.v[:, k, 128:144], k == 0, k == 15, [b_xn[k]])
                    I("act", "activation", [b_ps[bk]], [b_iw], out=iwtm[:, tb, :], in_=ps[:, bk, 0:16], func=AF.Copy, scale=0.03125)
                for hp in range(4):
                    pq = wpanel(w_in, 0, 16, C_IQ + hp * 256, 256)
                    for j in range(2):
                        c = hp * 2 + j
                        bk = proj_chunk(pq, j, xnT, b_xn)
                        I("act", "activation", [b_ps[bk]], [b_iq[c]], out=iqT[:, c, :], in_=ps[:, bk, :], func=AF.Copy)

                def gen_qproj():
                    for hp in range(8):
                        pq = wpanel(w_in, 0, 16, C_Q + hp * 256, 256)
                        for j in range(2):
                            c = hp * 2 + j
                            bk = proj_chunk(pq, j, xnT, b_xn)
                            I("act", "activation", [b_ps[bk]], [b_q[c]], out=qT[:, c, :], in_=ps[:, bk, :], func=AF.Copy,
                              scale=float(128 ** -0.5))
                            yield 4.5

                def gen_sctk(jj):
                    jb = ti * 4 + jj
                    Sj = (jb + 1) * 128
                    nch = (Sj + 511) // 512
                    mT = maskT[:, jj % 2]
                    bmT = b_maskT[jj % 2]
                    for h in range(16):
                        pb = (h % 2) * 64
                        for cc in range(nch):
                            w = min(512, Sj - cc * 512)
                            bk = bank()
                            I("pe", "matmul", [b_iq[h // 2], b_ik], [b_ps[bk]], ps[:, bk, 0:w],
                              lhsT=iqT[pb:pb + 64, h // 2, jj * 128:(jj + 1) * 128], rhs=ikT2[pb:pb + 64, cc * 512:cc * 512 + w],
                              start=True, stop=True)
                            k2 = (h * nch + cc) % 2
                            if (h * nch + cc) % 3 == 2:
                                I("dve", "tensor_scalar", [b_ps[bk]], [b_sgt[k2]], out=sgt[:, k2, 0:w], in0=ps[:, bk, 0:w], scalar1=0.0,
                                  scalar2=None, op0=ALU.max)
                            else:
                                I("act", "activation", [b_ps[bk]], [b_sgt[k2]], out=sgt[:, k2, 0:w], in_=ps[:, bk, 0:w], func=AF.Relu)
                            if h == 0:
                                I("dve", "tensor_scalar", [b_sgt[k2], b_iw], [b_acc[cc]], out=acc[:, cc * 512:cc * 512 + w],
                                  in0=sgt[:, k2, 0:w], scalar1=iwtm[:, jj, 0:1], scalar2=None, op0=ALU.mult)
                            else:
                                I("dve", "scalar_tensor_tensor", [b_sgt[k2], b_iw, b_acc[cc]], [b_acc[cc]],
                                  out=acc[:, cc * 512:cc * 512 + w], in0=sgt[:, k2, 0:w], scalar=iwtm[:, jj, h:h + 1],
                                  in1=acc[:, cc * 512:cc * 512 + w], op0=ALU.mult, op1=ALU.add)
                            yield 0.6
                    if jb >= 2:
                        I("dve", "reduce_max", b_acc, [b_bR], out=bis[:, 0:1], in_=acc[:, 0:Sj], axis=AX.X, apply_absolute_value=True)
                        I("dve", "tensor_scalar", [b_bR], [b_bRp], out=bis[:, 1:2], in0=bis[:, 0:1], scalar1=1.0009765625, scalar2=1e-30,
                          op0=ALU.mult, op1=ALU.add)
                        I("dve", "tensor_scalar", [b_bRp], [b_blo], out=bis[:, 2:3], in0=bis[:, 1:2], scalar1=-1.0, scalar2=None,
                          op0=ALU.mult)
                    I("dve", "tensor_tensor", b_acc + [b_causal], b_acc, out=acc[:, jb * 128:(jb + 1) * 128],
                      in0=acc[:, jb * 128:(jb + 1) * 128], in1=causal[:], op=ALU.add)
                    if jb >= 2:
                        for k in range(NPASS):
                            sk = 2.0 ** -k
                            I("dve", "scalar_tensor_tensor", [b_bRp, b_blo], [b_bnm], out=bis[:, 3:4], in0=bis[:, 1:2], scalar=-sk,
                              in1=bis[:, 2:3], op0=ALU.mult, op1=ALU.subtract)
                            I("act", "activation", b_acc + [b_bnm], [b_mask, b_bcnt], out=mask[:, 0:Sj], in_=acc[:, 0:Sj], func=AF.Sign,
                              bias=bis[:, 3:4], scale=1.0, accum_out=bis[:, 4:5])
                            I("dve", "tensor_scalar", [b_bcnt], [b_bd], out=bis[:, 5:6], in0=bis[:, 4:5], scalar1=511.5 - Sj, scalar2=sk,
                              op0=ALU.is_ge, op1=ALU.mult)
                            I("dve", "scalar_tensor_tensor", [b_bd, b_bRp, b_blo], [b_blo], out=bis[:, 2:3], in0=bis[:, 5:6],
                              scalar=bis[:, 1:2], in1=bis[:, 2:3], op0=ALU.mult, op1=ALU.add)
                            yield Sj / 1200.0 + 1.2
                        I("dve", "tensor_scalar", b_acc + [b_blo], [b_mask], out=mask[:, 0:Sj], in0=acc[:, 0:Sj], scalar1=bis[:, 2:3],
                          scalar2=None, op0=ALU.is_ge)
                    else:
                        I("dve", "tensor_scalar", b_acc, [b_mask], out=mask[:, 0:Sj], in0=acc[:, 0:Sj], scalar1=0.5 * NEG,
                          scalar2=None, op0=ALU.is_ge)
                    for i0 in range(0, jb + 1, 8):
                        n = min(8, jb + 1 - i0)
                        for i in range(n):
                            I("pe", "transpose", [b_mask, b_const], [b_psb], out=psb[:, i * 128:(i + 1) * 128],
                              in_=mask[:, (i0 + i) * 128:(i0 + i + 1) * 128], identity=ident_b[:])
                        I("act", "activation", [b_psb, b_const], [bmT], out=mT[:, i0:i0 + n, :],
                          in_=psb[:, 0:n * 128].rearrange("p (i t) -> p i t", i=n), func=AF.Identity, scale=30000.0, bias=c30k[:, 0:1])
                    yield 1.0

                def cost_sctk(jj):
                    jb = ti * 4 + jj
                    Sj = (jb + 1) * 128
                    nch = (Sj + 511) // 512
                    return 16 * nch * 0.6 + (NPASS * (Sj / 1200.0 + 1.2) if jb >= 2 else 0.0) + 1.0

                def gen_att(jj):
                    jb = ti * 4 + jj
                    mT = maskT[:, jj % 2]
                    bmT = b_maskT[jj % 2]
                    DEPTH = 2
                    pend = []

                    def qk(g, i):
                        dl = jb - i
                        bk = bank()
                        I("pe", "matmul", [b_kT] + b_q[4 * g:4 * g + 4], [b_ps[bk]],
                          ps[:, bk, :].rearrange("p (h t) -> p h t", h=4), lhsT=kT[:, g, i * 128:(i + 1) * 128],
                          rhs=qT[:, 4 * g:4 * g + 4, jj * 128:(jj + 1) * 128], start=True, stop=False)
                        for hh in range(4):
                            I("pe", "matmul", [bmT, b_const], [b_ps[bk]], ps[:, bk, hh * 128:(hh + 1) * 128], lhsT=ident_b[:],
                              rhs=mT[:, i, :], start=False, stop=(hh == 3))
                        k2 = pcnt[0] % 4
                        pcnt[0] += 1
                        if dl >= 2:
                            I("act", "activation", [b_ps[bk]], [b_pT[k2]], out=pT[:, k2, :], in_=ps[:, bk, :], func=AF.Exp)
                        else:
                            I("act", "activation", [b_ps[bk]], [b_e32], out=e32[:], in_=ps[:, bk, :], func=AF.Exp)
                            I("dve", "tensor_tensor", [b_e32, b_EB], [b_pT[k2]], out=pT[:, k2, :].rearrange("p (h t) -> p h t", h=4),
                              in0=e32[:].rearrange("p (h t) -> p h t", h=4), in1=EB[:, dl, 4 * g:4 * g + 4, :], op=ALU.mult)
                        return k2

                    def flush_one():
                        g, i, k2 = pend.pop(0)
                        I("pe", "matmul", [b_pT[k2], b_V], [b_ps[5]], ps[:, 5, :], lhsT=Vtm[:, i, g * 128:(g + 1) * 128],
                          rhs=pT[:, k2, :], start=(i == 0), stop=(i == jb))
                        I("pe", "matmul", [b_pT[k2], b_const], [b_ps[6]], ps[:, 6, :], lhsT=ones_b[:], rhs=pT[:, k2, :],
                          start=(i == 0), stop=(i == jb))
                        if i == jb:
                            I("dve", "reciprocal", [b_ps[6]], [b_rs], out=rs[:], in_=ps[:, 6, :])
                            I("dve", "tensor_tensor", [b_ps[5], b_rs], b_ya[4 * g:4 * g + 4],
                              out=yattnT[:, 4 * g:4 * g + 4, jj * 128:(jj + 1) * 128],
                              in0=ps[:, 5, :].rearrange("p (h t) -> p h t", h=4), in1=rs[:].rearrange("p (h t) -> p h t", h=4),
                              op=ALU.mult)

                    for g in range(4):
                        for i in range(jb + 1):
                            pend.append((g, i, qk(g, i)))
                            if len(pend) > DEPTH:
                                flush_one()
                            yield 0.9
                    while pend:
                        flush_one()
                    yield 1.2

                def cost_att(jj):
                    jb = ti * 4 + jj
                    return 4 * (jb + 1) * 0.9 + 1.2

                def interleave(ga, ta, gb, tb):
                    da = db = 0.0
                    la, lb = ga is not None, gb is not None
                    while la or lb:
                        if la and (not lb or da / ta <= db / tb):
                            try:
                                da += next(ga)
                            except StopIteration:
                                la = False
                        else:
                            try:
                                db += next(gb)
                            except StopIteration:
                                lb = False

                interleave(gen_sctk(0), cost_sctk(0), gen_qproj(), 72.0)
                for jj in range(1, 4):
                    interleave(gen_sctk(jj), cost_sctk(jj), gen_att(jj - 1), cost_att(jj - 1))
                interleave(None, 1.0, gen_att(3), cost_att(3))
                S.retire(allb)
            if dbg and ti == 0:
                dump(1, yattnT, b_ya, True)

            yrnnT = es2.enter_context(nc.sbuf_tensor(uq("yrnnT"), [128, 16, T], BF16))
            b_yr = [S.fresh() for _ in range(16)]
            with ExitStack() as ea:
                def sa(name, shape, dt):
                    return ea.enter_context(nc.sbuf_tensor(uq(name), list(shape), dt))
                rxp = sa("rxp", [128, 2, T + 3], F32)
                xc = sa("xc", [128, 2, T], F32)
                xcb = sa("xcb", [128, 2, T], BF16)
                rr = sa("rr", [128, 2, T], F32)
                ig = sa("ig", [128, 2, T], F32)
                aa = sa("aa", [128, 2, T], F32)
                a2 = sa("a2", [128, 2, T], F32)
                uu = sa("uu", [128, 2, T], F32)
                h2 = sa("h2", [128, 2, T], F32)
                gl = sa("gl", [128, 2, T], F32)
                b_rxp = [S.fresh(), S.fresh()]
                b_xc = [S.fresh(), S.fresh()]
                b_xcb = [S.fresh(), S.fresh()]
                b_rr, b_ig, b_aa, b_a2, b_uu = ([S.fresh(), S.fresh()] for _ in range(5))
                b_h2 = [S.fresh(), S.fresh()]
                b_gl = [S.fresh(), S.fresh()]
                allb = b_rxp + b_xc + b_xcb + b_rr + b_ig + b_aa + b_a2 + b_uu + b_h2 + b_gl
                for p in range(8):
                    pw = Panel()
                    s_ = rot["slot"]
                    rot["slot"] = (s_ + 1) % NSLOT
                    slot_gen[s_] += 1
                    pw.slot, pw.gen, pw.buf = s_, slot_gen[s_], b_slot[s_]
                    pw.v = wslot[s_][:, 0:512].rearrange("p (k n) -> p k n", n=128)
                    DMA("pool", slot_sems[s_], [], [b_slot[s_]], pw.v[:, 0:2, :], rgwa[2 * p:2 * p + 2].rearrange("n c d -> c n d"))
                    DMA("pool", slot_sems[s_], [], [b_slot[s_]], pw.v[:, 2:4, :], rgwx[2 * p:2 * p + 2].rearrange("n c d -> c n d"))
                    prx = wpanel(w_in, 0, 16, C_RX + p * 256, 256)
                    prg = wpanel(w_in, 0, 16, C_RG + p * 256, 256)
                    for j in range(2):
                        bk = proj_chunk(prx, j, xnT, b_xn)
                        I("act", "activation", [b_ps[bk]], [b_rxp[j]], out=rxp[:, j, 3:T + 3], in_=ps[:, bk, :], func=AF.Copy)
                    for j in range(2):
                        bk = proj_chunk(prg, j, xnT, b_xn)
                        I("act", "activation", [b_ps[bk]], [b_gl[j]], out=gl[:, j, :], in_=ps[:, bk, :], func=AF.Gelu_apprx_tanh)
                    for j in range(2):
                        c = p * 2 + j
                        I("dve", "tensor_copy", [b_halo], [b_rxp[j]], out=rxp[:, j, 0:3], in_=rxhalo[:, c, :])
                        I("dve", "tensor_copy", [b_rxp[j]], [b_halo], out=rxhalo[:, c, :], in_=rxp[:, j, T:T + 3])
                        I("dve", "tensor_scalar", [b_rxp[j], b_vecs], [b_xc[j]], out=xc[:, j, :], in0=rxp[:, j, 0:T],
                          scalar1=vecs[:, 4, c:c + 1], scalar2=vecs[:, 8, c:c + 1], op0=ALU.mult, op1=ALU.add)
                        for k in range(1, 4):
                            I("dve", "scalar_tensor_tensor", [b_rxp[j], b_vecs, b_xc[j]], [b_xc[j]], out=xc[:, j, :],
                              in0=rxp[:, j, k:k + T], scalar=vecs[:, 4 + k, c:c + 1], in1=xc[:, j, :], op0=ALU.mult, op1=ALU.add)
                        I("dve", "tensor_copy", [b_xc[j]], [b_xcb[j]], out=xcb[:, j, :], in_=xc[:, j, :])
                    gbk = []
                    for j in range(2):
                        bkr, bki = bank(), bank()
                        gbk.append((bkr, bki))
                        mm(bkr, T, pw, pw.v[:, j, :], xcb[:, j, :], True, True, [b_xcb[j]])
                        mm(bki, T, pw, pw.v[:, 2 + j, :], xcb[:, j, :], True, True, [b_xcb[j]])
                    for j in range(2):
                        c = p * 2 + j
                        bkr, bki = gbk[j]
                        I("act", "activation", [b_ps[bkr], b_vecs], [b_rr[j]], out=rr[:, j, :], in_=ps[:, bkr, :], func=AF.Sigmoid,
                          bias=vecs[:, 9, c:c + 1])
                        I("act", "activation", [b_ps[bki], b_vecs], [b_ig[j]], out=ig[:, j, :], in_=ps[:, bki, :], func=AF.Sigmoid,
                          bias=vecs[:, 10, c:c + 1])
                    for j in range(2):
                        c = p * 2 + j
                        I("act", "activation", [b_rr[j], b_nsp], [b_aa[j]], out=aa[:, j, :], in_=rr[:, j, :], func=AF.Exp, scale=nsp8[:, c:c + 1])
                        I("act", "activation", [b_rr[j], b_nsp], [b_a2[j]], out=a2[:, j, :], in_=rr[:, j, :], func=AF.Exp, scale=nsp16[:, c:c + 1])
                    for j in range(2):
                        I("act", "activation", [b_a2[j], b_const], [b_a2[j]], out=a2[:, j, :], in_=a2[:, j, :], func=AF.Sqrt, scale=-1.0,
                          bias=ones_f[:, 0:1])
                    for j in range(2):
                        c = p * 2 + j
                        I("dve", "tensor_tensor", [b_ig[j], b_xc[j]], [b_uu[j]], out=uu[:, j, :], in0=ig[:, j, :], in1=xc[:, j, :], op=ALU.mult)
                        I("dve", "tensor_tensor", [b_uu[j], b_a2[j]], [b_uu[j]], out=uu[:, j, :], in0=uu[:, j, :], in1=a2[:, j, :], op=ALU.mult)
                        I("dve", "tensor_tensor_scan", [b_aa[j], b_uu[j], b_hcar], [b_h2[j]], out=h2[:, j, :], data0=aa[:, j, :], data1=uu[:, j, :],
                          initial=hcar[:, c:c + 1], op0=ALU.mult, op1=ALU.add)
                        I("dve", "tensor_copy", [b_h2[j]], [b_hcar], out=hcar[:, c:c + 1], in_=h2[:, j, T - 1:T])
                    for j in range(2):
                        c = p * 2 + j
                        I("dve", "tensor_tensor", [b_gl[j], b_h2[j]], [b_yr[c]], out=yrnnT[:, c, :], in0=gl[:, j, :], in1=h2[:, j, :],
                          op=ALU.mult)
                S.retire(allb)
            if dbg and ti == 0:
                dump(2, yrnnT, b_yr, True)

            with ExitStack() as ea:
                def sa(name, shape, dt):
                    return ea.enter_context(nc.sbuf_tensor(uq(name), list(shape), dt))
                merged = sa("merged", [128, 16, T], BF16)
                sgr = sa("sgr", [128, 2, T], F32)
                sga = sa("sga", [128, 2, T], F32)
                tm1 = sa("tm1", [128, T], F32)
                tm2 = sa("tm2", [128, T], F32)
                b_mg = [S.fresh() for _ in range(16)]
                b_sgr = [S.fresh(), S.fresh()]
                b_sga = [S.fresh(), S.fresh()]
                b_tm1, b_tm2 = S.fresh(), S.fresh()
                allb = b_mg + b_sgr + b_sga + [b_tm1, b_tm2]
                for p in range(8):
                    pgr = wpanel(w_in, 0, 16, C_GR + p * 256, 256)
                    for j in range(2):
                        bk = proj_chunk(pgr, j, xnT, b_xn)
                        I("act", "activation", [b_ps[bk]], [b_sgr[j]], out=sgr[:, j, :], in_=ps[:, bk, :], func=AF.Sigmoid)
                    pga = wpanel(w_in, 0, 16, C_GA + p * 256, 256)
                    for j in range(2):
                        bk = proj_chunk(pga, j, xnT, b_xn)
                        I("act", "activation", [b_ps[bk]], [b_sga[j]], out=sga[:, j, :], in_=ps[:, bk, :], func=AF.Sigmoid)
                    ppr = wpanel(wpr, 0, 16, p * 256, 256)
                    ppa = wpanel(wpa, 0, 16, p * 256, 256)
                    for j in range(2):
                        c = p * 2 + j
                        b1 = proj_chunk(ppr, j, yrnnT, b_yr)
                        b2 = proj_chunk(ppa, j, yattnT, b_ya)
                        I("dve", "tensor_tensor", [b_ps[b1], b_sgr[j]], [b_tm1], out=tm1[:], in0=sgr[:, j, :], in1=ps[:, b1, :], op=ALU.mult)
                        I("dve", "tensor_tensor", [b_ps[b2], b_sga[j]], [b_tm2], out=tm2[:], in0=sga[:, j, :], in1=ps[:, b2, :], op=ALU.mult)
                        I("dve", "tensor_tensor", [b_tm1, b_tm2], [b_mg[c]], out=merged[:, c, :], in0=tm1[:], in1=tm2[:], op=ALU.add)
                for p in range(8):
                    po = wpanel(wout, 0, 16, p * 256, 256)
                    for j in range(2):
                        c = p * 2 + j
                        bk = proj_chunk(po, j, merged, b_mg)
                        I("dve", "tensor_tensor", [b_ps[bk], b_xT[c]], [b_xT[c]], out=xT[:, c, :], in0=xT[:, c, :], in1=ps[:, bk, :],
                          op=ALU.add)
                S.retire(allb)
            S.retire(b_ya + b_yr)
            es2.close()

            rmsnorm(2, False)
            ffn(w2g, w2u, w2d)
            rmsnorm(3, True)

            with nc.sbuf_tensor(uq("otm"), [128, 2, D], F32) as otm:
                b_otm = [S.fresh(), S.fresh()]
                for tb in range(4):
                    k2 = tb % 2
                    for cg in range(4):
                        bk = bank()
                        for j in range(4):
                            c = cg * 4 + j
                            I("pe", "transpose", [b_xT[c], b_const], [b_ps[bk]], out=ps[:, bk, j * 128:(j + 1) * 128],
                              in_=xT[:, c, tb * 128:(tb + 1) * 128], identity=ident_f[:])
                        I("act", "activation", [b_ps[bk]], [b_otm[k2]], out=otm[:, k2, cg * 512:(cg + 1) * 512], in_=ps[:, bk, :],
                          func=AF.Copy)
                    ob = Buf()
                    out_bufs.append(ob)
                    DMA("sp", d_out[k2], [b_otm[k2]], [ob], out_d[t0 + tb * 128:t0 + (tb + 1) * 128, :], otm[:, k2, :])
                S.retire(b_otm)

        I("sp", "nop", out_bufs, [])
        S.emit(block, esems)
    return nc


_BUCKET = t5_bucket_table()


def _prep_inputs(inp):
    f32 = np.float32

    def fm(v):
        return np.ascontiguousarray(np.asarray(v, f32).reshape(16, 128).T)

    vec_list = [inp["ffn1_norm"][0], inp["mix_norm"][0], inp["ffn2_norm"][0], inp["final_norm"],
                inp["conv_w"][0][0], inp["conv_w"][0][1], inp["conv_w"][0][2], inp["conv_w"][0][3],
                inp["conv_b"][0], inp["rg_b_a"][0], inp["rg_b_x"][0], inp["rg_lambda"][0]]
    vecs = np.ascontiguousarray(np.stack([fm(v) for v in vec_list], axis=1))
    rb = np.asarray(inp["rel_bias"], f32)
    biasT = np.ascontiguousarray(np.transpose(rb[_BUCKET], (1, 0, 3, 2)))
    cvec = np.ascontiguousarray(np.broadcast_to(rb[31][None, :], (128, 16)))
    s = np.arange(128)
    causal = np.where(s[None, :] <= s[:, None], 0.0, NEG).astype(f32)
    shared = {
        "ffn1_w_gate": np.asarray(inp["ffn1_w_gate"][0], f32), "ffn1_w_up": np.asarray(inp["ffn1_w_up"][0], f32),
        "ffn1_w_down": np.asarray(inp["ffn1_w_down"][0], f32),
        "ffn2_w_gate": np.asarray(inp["ffn2_w_gate"][0], f32), "ffn2_w_up": np.asarray(inp["ffn2_w_up"][0], f32),
        "ffn2_w_down": np.asarray(inp["ffn2_w_down"][0], f32),
        "w_in": np.asarray(inp["w_in"][0], f32), "rg_w_a": np.asarray(inp["rg_w_a"][0], f32), "rg_w_x": np.asarray(inp["rg_w_x"][0], f32),
        "w_proj_rnn": np.asarray(inp["w_proj_rnn"][0], f32), "w_proj_attn": np.asarray(inp["w_proj_attn"][0], f32),
        "w_out": np.asarray(inp["w_out"][0], f32),
        "vecs": vecs, "biasT": biasT.astype(f32), "cvec": cvec.astype(f32), "ident": np.eye(128, dtype=f32), "causal": causal,
    }
    return shared


def kernel(**inputs):
    ntiles = int(os.environ.get("KNT", NT))
    dbg = bool(int(os.environ.get("KDBG", "0")))
    shared = _prep_inputs(inputs)
    x = np.asarray(inputs["x"], np.float32)
    nc = build(ntiles, dbg)
    in_maps = []
    for b in range(8):
        m = dict(shared)
        m["x"] = np.ascontiguousarray(x[b])
        in_maps.append(m)
    res = run_bass_kernel_spmd(nc, in_maps, core_ids=list(range(8)))
    out = np.stack([np.asarray(r["out"], np.float32) for r in res.results], axis=0)
    if dbg:
        kernel.dbg = res.results[0]
    return out
```
